# Optimizing a Trainium2 kernel written in Bass

```python
import math
import jax, jax.numpy as jnp
from jax import lax
import numpy as np

D_MODEL = 4096
BATCH = 4
SEQ = 4096
DEPTH = 1

HEAD_DIM = 128
SB_HEADS = 16
FX_HEADS = 16
SB_WIDTH = SB_HEADS * HEAD_DIM
FX_WIDTH = FX_HEADS * HEAD_DIM
N_BRANCHES = 2
Q_BLOCK = 128
SPLIT_SIZES = [SB_WIDTH] * 3 + [FX_WIDTH] * 3 + [FX_HEADS, N_BRANCHES * D_MODEL]
SPLIT_OFFSETS = [int(o) for o in np.cumsum(SPLIT_SIZES)[:-1]]
PROJ_WIDTH = int(sum(SPLIT_SIZES))
N_EXPERTS = 32
TOP_K = 4
D_FF = D_MODEL // 2
SWIGLU_ALPHA = 1.702
SWIGLU_LIMIT = 7.0
EXPERT_BLOCK = 512
LN_EPS = 1e-5
DEEPNORM_ALPHA = (2.0 * DEPTH) ** 0.25
DEEPNORM_BETA = (8.0 * DEPTH) ** -0.25

kernel_name = "stickbreak_fox_gated_moe_deepnorm"


def layer_norm(x, g, b):
    xf = x.astype(jnp.float32)
    mu = jnp.mean(xf, axis=-1, keepdims=True)
    var = jnp.mean(jnp.square(xf - mu), axis=-1, keepdims=True)
    y = (xf - mu) * lax.rsqrt(var + LN_EPS)
    return (y * g.astype(jnp.float32) + b.astype(jnp.float32)).astype(x.dtype)


def to_heads(t, n_heads):
    b, s, _ = t.shape
    return t.reshape(b, s, n_heads, HEAD_DIM).transpose(0, 2, 1, 3)


def to_query_blocks(t):
    b, h, s = t.shape[:3]
    nb = s // Q_BLOCK
    t = t.reshape((b, h, nb, Q_BLOCK) + t.shape[3:])
    return jnp.moveaxis(t, 2, 0)


def from_query_blocks(o):
    nb, b, h, qb, d = o.shape
    return o.transpose(1, 0, 3, 2, 4).reshape(b, nb * qb, h * d)


def stick_breaking_attention(q, k, v):
    s_len = q.shape[2]
    scale = HEAD_DIM ** -0.5
    kpos = jnp.arange(s_len)
    starts = jnp.arange(s_len // Q_BLOCK) * Q_BLOCK

    def block(args):
        qb, start = args
        qpos = start + jnp.arange(Q_BLOCK)
        strict = kpos[None, :] < qpos[:, None]
        z = jnp.einsum('bhqd,bhkd->bhqk', qb, k).astype(jnp.float32) * scale
        log_beta = jax.nn.log_sigmoid(z)
        log_keep = jnp.where(strict, jax.nn.log_sigmoid(-z), 0.0)
        after = lax.cumsum(log_keep, axis=3, reverse=True) - log_keep
        w = jnp.where(strict, jnp.exp(log_beta + after), 0.0)
        return jnp.einsum('bhqk,bhkd->bhqd', w.astype(v.dtype), v)

    return from_query_blocks(lax.map(block, (to_query_blocks(q), starts)))


def forgetting_attention(q, k, v, log_f):
    s_len = q.shape[2]
    scale = HEAD_DIM ** -0.5
    kpos = jnp.arange(s_len)
    starts = jnp.arange(s_len // Q_BLOCK) * Q_BLOCK
    c = jnp.cumsum(log_f, axis=-1)

    def block(args):
        qb, cq, start = args
        qpos = start + jnp.arange(Q_BLOCK)
        causal = kpos[None, :] <= qpos[:, None]
        z = jnp.einsum('bhqd,bhkd->bhqk', qb, k).astype(jnp.float32) * scale
        logits = z + cq[..., None] - c[:, :, None, :]
        p = jax.nn.softmax(jnp.where(causal, logits, -jnp.inf), axis=-1)
        return jnp.einsum('bhqk,bhkd->bhqd', p.astype(v.dtype), v)

    return from_query_blocks(lax.map(block, (to_query_blocks(q), to_query_blocks(c), starts)))


def token_mixer(x, w_in, b_in, w_branch_sb, w_branch_fx, w_out):
    proj = x @ w_in + b_in
    q_sb, k_sb, v_sb, q_fx, k_fx, v_fx, f_logit, gates = jnp.split(proj, SPLIT_OFFSETS, axis=-1)
    o_sb = stick_breaking_attention(to_heads(q_sb, SB_HEADS), to_heads(k_sb, SB_HEADS),
                                    to_heads(v_sb, SB_HEADS))
    log_f = jax.nn.log_sigmoid(f_logit.astype(jnp.float32)).transpose(0, 2, 1)
    o_fx = forgetting_attention(to_heads(q_fx, FX_HEADS), to_heads(k_fx, FX_HEADS),
                                to_heads(v_fx, FX_HEADS), log_f)
    g_sb, g_fx = jnp.split(jax.nn.sigmoid(gates), N_BRANCHES, axis=-1)
    merged = g_sb * (o_sb @ w_branch_sb) + g_fx * (o_fx @ w_branch_fx)
    return merged @ w_out


def moe(h, w_router, b_router, w_up, b_up, w_down, b_down):
    b, s, d = h.shape
    n_tok = b * s
    n_assign = n_tok * TOP_K
    tok = h.reshape(n_tok, d)
    logits = (tok @ w_router + b_router).astype(jnp.float32)
    top_vals, top_idx = lax.top_k(logits, TOP_K)
    gate = jax.nn.softmax(top_vals, axis=-1)

    flat_e = top_idx.reshape(-1)
    flat_tok = jnp.repeat(jnp.arange(n_tok), TOP_K)
    flat_gate = gate.reshape(-1)
    order = jnp.argsort(flat_e)
    sorted_e = flat_e[order]
    sorted_tok = flat_tok[order]
    sorted_gate = flat_gate[order]

    counts = jnp.bincount(flat_e, length=N_EXPERTS)
    padded = (counts + EXPERT_BLOCK - 1) // EXPERT_BLOCK * EXPERT_BLOCK
    start = jnp.cumsum(counts) - counts
    pend = jnp.cumsum(padded)
    pstart = pend - padded
    dest = pstart[sorted_e] + (jnp.arange(n_assign) - start[sorted_e])

    n_blk = -(-n_assign // EXPERT_BLOCK) + N_EXPERTS
    n_rows = n_blk * EXPERT_BLOCK
    rows = jnp.zeros((n_rows, d), tok.dtype).at[dest].set(tok[sorted_tok])
    blk_start = jnp.arange(n_blk) * EXPERT_BLOCK
    blk_e = jnp.clip(jnp.searchsorted(pend, blk_start, side='right'), 0, N_EXPERTS - 1)

    def expert_block(args):
        xb, e = args
        hu = xb @ w_up[e] + b_up[e]
        g, u = hu[:, :D_FF], hu[:, D_FF:]
        g = jnp.minimum(g, SWIGLU_LIMIT)
        u = jnp.clip(u, -SWIGLU_LIMIT, SWIGLU_LIMIT)
        act = g * jax.nn.sigmoid(SWIGLU_ALPHA * g) * (u + 1.0)
        return act @ w_down[e] + b_down[e]

    y_rows = lax.map(expert_block, (rows.reshape(n_blk, EXPERT_BLOCK, d), blk_e)).reshape(n_rows, d)
    y_assign = y_rows[dest] * sorted_gate[:, None].astype(y_rows.dtype)
    out = jax.ops.segment_sum(y_assign, sorted_tok, num_segments=n_tok)
    return out.reshape(b, s, d)


def setup_inputs(seed: int = 0) -> dict:
    key = jax.random.key(seed)
    ks = jax.random.split(key, 20)
    f32 = jnp.float32
    nrm = lambda k, shape, scale: jax.random.normal(k, shape, f32) * scale
    x = jax.random.normal(ks[0], (BATCH, SEQ, D_MODEL), f32)
    w_in = nrm(ks[1], (DEPTH, D_MODEL, PROJ_WIDTH), D_MODEL ** -0.5)
    b_in = jnp.concatenate([
        nrm(ks[2], (DEPTH, 3 * SB_WIDTH + 3 * FX_WIDTH), 0.01),
        jax.random.uniform(ks[3], (DEPTH, FX_HEADS), f32, minval=1.0, maxval=5.0),
        nrm(ks[4], (DEPTH, N_BRANCHES * D_MODEL), 0.01),
    ], axis=-1)
    w_branch_sb = nrm(ks[5], (DEPTH, SB_WIDTH, D_MODEL), SB_WIDTH ** -0.5)
    w_branch_fx = nrm(ks[6], (DEPTH, FX_WIDTH, D_MODEL), FX_WIDTH ** -0.5)
    w_out = nrm(ks[7], (DEPTH, D_MODEL, D_MODEL), DEEPNORM_BETA * D_MODEL ** -0.5)
    ln1_g = 1.0 + nrm(ks[8], (DEPTH, D_MODEL), 0.02)
    ln1_b = nrm(ks[9], (DEPTH, D_MODEL), 0.01)
    w_router = nrm(ks[10], (DEPTH, D_MODEL, N_EXPERTS), D_MODEL ** -0.5)
    b_router = nrm(ks[11], (DEPTH, N_EXPERTS), 0.01)
    w_up = nrm(ks[12], (DEPTH, N_EXPERTS, D_MODEL, 2 * D_FF), D_MODEL ** -0.5)
    b_up = nrm(ks[13], (DEPTH, N_EXPERTS, 2 * D_FF), 0.01)
    w_down = nrm(ks[14], (DEPTH, N_EXPERTS, D_FF, D_MODEL), DEEPNORM_BETA * D_FF ** -0.5)
    b_down = nrm(ks[15], (DEPTH, N_EXPERTS, D_MODEL), 0.01)
    ln2_g = 1.0 + nrm(ks[16], (DEPTH, D_MODEL), 0.02)
    ln2_b = nrm(ks[17], (DEPTH, D_MODEL), 0.01)
    return {"x": x, "w_in": w_in, "b_in": b_in, "w_branch_sb": w_branch_sb,
            "w_branch_fx": w_branch_fx, "w_out": w_out, "ln1_g": ln1_g, "ln1_b": ln1_b,
            "w_router": w_router, "b_router": b_router, "w_up": w_up, "b_up": b_up,
            "w_down": w_down, "b_down": b_down, "ln2_g": ln2_g, "ln2_b": ln2_b}


def reference(x, w_in, b_in, w_branch_sb, w_branch_fx, w_out, ln1_g, ln1_b,
              w_router, b_router, w_up, b_up, w_down, b_down, ln2_g, ln2_b):
    h = x
    for l in range(DEPTH):
        mix = token_mixer(h, w_in[l], b_in[l], w_branch_sb[l], w_branch_fx[l], w_out[l])
        h = layer_norm(DEEPNORM_ALPHA * h + mix, ln1_g[l], ln1_b[l])
        ffn = moe(h, w_router[l], b_router[l], w_up[l], b_up[l], w_down[l], b_down[l])
        h = layer_norm(DEEPNORM_ALPHA * h + ffn, ln2_g[l], ln2_b[l])
    return h
```

```python
import numpy as np
import ml_dtypes
from contextlib import ExitStack
import concourse.bass as bass
import concourse.mybir as mybir
from concourse.bass_utils import run_bass_kernel_spmd

F32, BF16, I32 = mybir.dt.float32, mybir.dt.bfloat16, mybir.dt.int32
AF = mybir.ActivationFunctionType
ALU = mybir.AluOpType
AX = mybir.AxisListType

NCORES = 4
DEBUG_NAMES = ('s_dbg', 's_dbg2', 's_dbg3', 's_xT', 's_qT', 's_kT', 's_v', 's_cfx', 's_cfxT', 's_oT', 's_mT', 's_h1_0', 's_hb_0', 's_xe', 's_ye', 's_wq')


class Cfg:
    def __init__(self, D=4096, S=4096, BL=4, H=16, E=32, CH=2048, CAP=384):
        self.D, self.S, self.BL, self.H, self.E, self.CH, self.CAP = D, S, BL, H, E, CH, CAP
        self.W = H * 128
        self.DFF = D // 2
        self.PROJ = 6 * self.W + H + 2 * D
        self.KC = D // 128
        self.T = BL * S
        self.G0 = 6 * self.W + H
        self.alpha = 2.0 ** 0.25
        self.eps = 1e-5


class Res:
    def __init__(self, name, obj=None):
        self.name, self.obj, self.w, self.r = name, obj, {}, {}
        self.dsem = None

    def __getitem__(self, idx):
        return self.obj[idx]


class Ctx:
    def __init__(self, nc, es):
        self.nc, self.es = nc, es
        self.eng = {'pe': nc.tensor, 'act': nc.scalar, 'dve': nc.vector, 'pool': nc.gpsimd, 'sp': nc.sync}
        self.psem = {k: es.enter_context(nc.semaphore('p_' + k)) for k in self.eng}
        self.pcnt = {k: 0 for k in self.eng}
        self.waited = {k: {} for k in self.eng}
        self.dsem = {}
        self.sem_all, self.sem_free = [], []
        self.nins = 0

    def borrow_sem(self):
        if self.sem_free:
            return self.sem_free.pop()
        key = 'd_%d' % len(self.sem_all)
        ent = [self.es.enter_context(self.nc.semaphore(key)), 0, key]
        self.sem_all.append(ent)
        return ent

    def _wait(self, e, events, own_ok=True):
        for name, (sem, val) in events.items():
            if own_ok and name == 'p_' + e and e in ('pe', 'sp'):
                continue
            if self.waited[e].get(name, 0) >= val:
                continue
            self.eng[e].wait_ge(sem, val)
            self.waited[e][name] = val

    def _deps(self, e, reads, writes, disjoint, own_ok=True):
        for r in reads:
            self._wait(e, r.w, own_ok)
        for w in writes:
            self._wait(e, w.r, own_ok)
            self._wait(e, w.w, own_ok)

    def _mark(self, ev, reads, writes, disjoint):
        for r in reads:
            r.r[ev[0]] = ev[1]
        for w in writes:
            if not disjoint:
                w.w = {}
                w.r = {}
            w.w[ev[0]] = ev[1]

    def op(self, e, fn, reads=(), writes=(), disjoint=False):
        self._deps(e, reads, writes, disjoint)
        ins = fn(self.eng[e])
        self.pcnt[e] += 1
        ins.then_inc(self.psem[e], 1)
        self._mark(('p_' + e, (self.psem[e], self.pcnt[e])), reads, writes, disjoint)
        self.nins += 1
        return ins

    def dma(self, q, stream, out, in_, reads=(), writes=(), disjoint=True, fn=None, **kw):
        side = [r_ for r_ in list(writes) + list(reads) if r_.obj is not None]
        assert side, stream
        owner = side[0]
        if owner.dsem is None:
            owner.dsem = self.borrow_sem()
        ent = owner.dsem
        key = ent[2]
        self._deps(q, reads, writes, disjoint, own_ok=False)
        if fn is None:
            ins = self.eng[q].dma_start(out=out, in_=in_, **kw)
        else:
            ins = fn(self.eng[q])
        ent[1] += 16
        ins.then_inc(ent[0], 16)
        self._mark((key, (ent[0], ent[1])), reads, writes, disjoint)
        self.nins += 1
        return ins

    def barrier(self):
        evs = {'p_' + k: (self.psem[k], self.pcnt[k]) for k in self.eng if self.pcnt[k] > 0}
        for ent in self.sem_all:
            if ent[1] > 0:
                evs[ent[2]] = (ent[0], ent[1])
        for e in self.eng:
            self._wait(e, evs)


class Pool_:
    def __init__(self, ctx):
        self.ctx, self.es = ctx, ExitStack()
        self.n = 0
        self.res = []

    def sb(self, shape, dt, name='t'):
        self.n += 1
        t = self.es.enter_context(self.ctx.nc.sbuf_tensor('%s_%d_%d' % (name, id(self) % 9973, self.n), list(shape), dt))
        r_ = Res(name, t)
        self.res.append(r_)
        return r_

    def ps(self, shape, dt, name='ps'):
        self.n += 1
        t = self.es.enter_context(self.ctx.nc.psum_tensor('%s_%d_%d' % (name, id(self) % 9973, self.n), list(shape), dt))
        return Res(name, t)

    def ring(self, n, shape, dt, name='r', psum=False):
        return Ring([(self.ps if psum else self.sb)(shape, dt, name) for _ in range(n)])

    def close(self):
        self.ctx.barrier()
        for r_ in self.res:
            if r_.dsem is not None:
                self.ctx.sem_free.append(r_.dsem)
                r_.dsem = None
        self.es.close()


class Ring:
    def __init__(self, items):
        self.items, self.i = items, -1

    def next(self):
        self.i = (self.i + 1) % len(self.items)
        return self.items[self.i]


def build(cfg, ncores, debug=False):
    c = cfg
    D, S, BL, H, E, W, DFF, KC, T, CAP, CH = c.D, c.S, c.BL, c.H, c.E, c.W, c.DFF, c.KC, c.T, c.CAP, c.CH
    nc = bass.Bass("TRN2", target_bir_lowering=False)
    dt_in = lambda n, s, d=F32: nc.dram_tensor(n, list(s), d, kind="ExternalInput").ap()
    x = dt_in("x", [T, D])
    w_in = dt_in("w_in", [D, c.PROJ])
    b_in = dt_in("b_in", [1, c.PROJ])
    w_bs = dt_in("w_branch_sb", [W, D])
    w_bf = dt_in("w_branch_fx", [W, D])
    w_out = dt_in("w_out", [D, D])
    ln1_g, ln1_b = dt_in("ln1_g", [1, D]), dt_in("ln1_b", [1, D])
    w_router, b_router = dt_in("w_router", [D, E]), dt_in("b_router", [1, E])
    w_up, b_up = dt_in("w_up", [E * D, 2 * DFF]), dt_in("b_up", [E, 2 * DFF])
    w_down, b_down = dt_in("w_down", [E * DFF, D]), dt_in("b_down", [E, D])
    ln2_g, ln2_b = dt_in("ln2_g", [1, D]), dt_in("ln2_b", [1, D])
    c_identb = dt_in("c_identb", [128, 128], BF16)
    c_identf = dt_in("c_identf", [128, 128])
    c_mfx = dt_in("c_mfx", [128, 4 * 512])
    c_msb = dt_in("c_msb", [128, 4 * 512])
    c_tri = dt_in("c_tri", [128, 3 * 128])
    c_ecap = dt_in("c_ecap", [128, E])
    out = nc.dram_tensor("out", [T, D], F32, kind="ExternalOutput").ap()

    dscr = lambda n, s, d: (nc.dram_tensor(n, list(s), d, kind='ExternalOutput').ap() if (debug and n in DEBUG_NAMES) else nc.dram_tensor(n, list(s), d).ap())
    wq = dscr("s_wq", [D, c.PROJ], BF16)
    wbs, wbf, wo = dscr("s_wbs", [W, D], BF16), dscr("s_wbf", [W, D], BF16), dscr("s_wo", [D, D], BF16)
    xT = dscr("s_xT", [D, S], BF16)
    qT, kT = dscr("s_qT", [2 * W, S], BF16), dscr("s_kT", [2 * W, S], BF16)
    vv = dscr("s_v", [S, 2 * W], BF16)
    cfx, cfxT = dscr("s_cfx", [S, H], F32), dscr("s_cfxT", [H, S], F32)
    oT = dscr("s_oT", [2 * W, S], BF16)
    dbg = dscr("s_dbg", [S, 3 * H], F32)
    dbg2 = dscr("s_dbg2", [128, 5 * 512], F32)
    dbg3 = dscr("s_dbg3", [128, 3 * 512], BF16)
    mT = dscr("s_mT", [D, S], BF16)
    h1 = [dscr("s_h1_%d" % b, [S, D], F32) for b in range(BL)]
    hb = [dscr("s_hb_%d" % b, [S, D], BF16) for b in range(BL)]

    es = ExitStack()
    ctx = Ctx(nc, es)
    R = lambda n: Res(n)
    r_w = R("weights")
    r_xT, r_q, r_k, r_v, r_c, r_oT, r_mT = R("xT"), R("qT"), R("kT"), R("v"), R("c"), R("oT"), R("mT")
    r_h1, r_hb, r_xe, r_ye, r_out = R("h1"), R("hb"), R("xe"), R("ye"), R("out")
    scale = 128 ** -0.5

    P0 = Pool_(ctx)
    identb, identf = P0.sb([128, 128], BF16, 'identb'), P0.sb([128, 128], F32, 'identf')
    tri = P0.sb([128, 384], F32, 'tri')
    trib = P0.sb([128, 384], BF16, 'trib')
    ctx.dma('sp', 'const', identb[:], c_identb[:, :], writes=[identb])
    ctx.dma('sp', 'const', identf[:], c_identf[:, :], writes=[identf])
    ctx.dma('sp', 'const', tri[:], c_tri[:, :], writes=[tri])
    ctx.op('dve', lambda e: e.tensor_copy(out=trib[:], in_=tri[:]), reads=[tri], writes=[trib])

    def cast_matrix(P, src, dst, rows, cols, rings, cnt):
        src_v = src.rearrange("(r p) c -> r p c", p=128)
        dst_v = dst.rearrange("(r p) c -> r p c", p=128)
        cw = min(cols, 4096)
        for r in range(rows // 128):
            for c0 in range(0, cols, cw):
                cc = min(cw, cols - c0)
                a, b_ = rings[0].next(), rings[1].next()
                ctx.dma('sp', 'cast_in', a[:, :cc], src_v[r, :, c0:c0 + cc], writes=[a], disjoint=False)
                e = ('dve', 'pool', 'act')[cnt[0] % 3]
                cnt[0] += 1
                if e == 'act':
                    ctx.op(e, lambda g: g.copy(out=b_[:, :cc], in_=a[:, :cc]), reads=[a], writes=[b_])
                else:
                    ctx.op(e, lambda g: g.tensor_copy(out=b_[:, :cc], in_=a[:, :cc]), reads=[a], writes=[b_])
                ctx.dma('act', 'cast_out', dst_v[r, :, c0:c0 + cc], b_[:, :cc], reads=[b_], writes=[r_w])

    P = Pool_(ctx)
    rings = (P.ring(3, [128, 4096], F32, 'ci'), P.ring(3, [128, 4096], BF16, 'co'))
    cnt = [0]
    cast_matrix(P, w_in, wq, D, c.PROJ, rings, cnt)
    cast_matrix(P, w_bs, wbs, W, D, rings, cnt)
    cast_matrix(P, w_bf, wbf, W, D, rings, cnt)
    cast_matrix(P, w_out, wo, D, D, rings, cnt)
    P.close()

    wq_v = wq.rearrange("(kc p) n -> p kc n", p=128)
    xT_v = xT.rearrange("(kc p) s -> p kc s", p=128)

    NB = (6 * W) // 128
    NG = (2 * D) // 128
    binT = P0.sb([128, NB], F32, 'binT')
    bgT = P0.sb([128, NG], F32, 'bgT')
    ctx.dma('sp', 'const', binT[:], b_in[0, 0:6 * W].rearrange("(t p) -> p t", p=128), writes=[binT],
            allow_slow_non_contiguous=True)
    ctx.dma('sp', 'const', bgT[:], b_in[0, c.G0:c.G0 + 2 * D].rearrange("(t p) -> p t", p=128), writes=[bgT],
            allow_slow_non_contiguous=True)

    for b in range(BL):
        xb_rows = x[b * S:(b + 1) * S, :]
        P = Pool_(ctx)
        xin = P.ring(2, [128, D], F32, 'xin')
        xbf = P.ring(2, [128, D], BF16, 'xbf')
        xTt = P.ring(2, [128, KC, 512], BF16, 'xTt')
        pst = P.ring(4, [128, 1024], BF16, 'pst', psum=True)
        for tb in range(S // 512):
            xt_ = xTt.next()
            for j in range(4):
                tt = tb * 4 + j
                a, bb = xin.next(), xbf.next()
                ctx.dma('sp', 'x_in', a[:], xb_rows[tt * 128:(tt + 1) * 128, :], writes=[a], disjoint=False)
                ctx.op('pool', lambda g: g.tensor_copy(out=bb[:], in_=a[:]), reads=[a], writes=[bb])
                for k4 in range(KC // 4):
                    ps = pst.next()
                    for kk in range(4):
                        kc = k4 * 4 + kk
                        ctx.op('pe', lambda g: g.transpose(out=ps[:, kk * 128:(kk + 1) * 128],
                                                           in_=bb[:, kc * 128:(kc + 1) * 128], identity=identb[:]),
                               reads=[bb, identb], writes=[ps], disjoint=(kk > 0))
                    eng = 'dve' if k4 % 2 == 0 else 'act'
                    src = ps[:, 0:512].rearrange("p (k t) -> p k t", k=4)
                    dst = xt_[:, k4 * 4:(k4 + 1) * 4, j * 128:(j + 1) * 128]
                    if eng == 'dve':
                        ctx.op('dve', lambda g: g.tensor_copy(out=dst, in_=src), reads=[ps], writes=[xt_],
                               disjoint=not (j == 0 and k4 == 0))
                    else:
                        ctx.op('act', lambda g: g.copy(out=dst, in_=src), reads=[ps], writes=[xt_], disjoint=True)
            ctx.dma('act', 'xT_out', xT_v[:, :, tb * 512:(tb + 1) * 512], xt_[:], reads=[xt_], writes=[r_xT])
        P.close()

        P = Pool_(ctx)
        xTb_r = P.ring(2, [128, KC, 512], BF16, 'xTb')
        wblk_r = P.ring(2, [128, KC, 512], BF16, 'wblk')
        psA = P.ring(4, [128, 512], F32, 'psA', psum=True)
        psF = P.ring(2, [128, 512], F32, 'psF', psum=True)
        oA = P.ring(3, [128, 512], BF16, 'oA')
        bv = P.sb([128, 2 * W], F32, 'bv')
        bf_ = P.sb([128, H], F32, 'bf')
        wf = P.sb([128, KC, 64], BF16, 'wf')
        carry = P.ring(2, [128, H], F32, 'carry')
        lf_r = P.ring(2, [128, H], F32, 'lf')
        lhi_r, llo_r = P.ring(2, [128, H], BF16, 'lhi'), P.ring(2, [128, H], BF16, 'llo')
        ctmp = P.ring(2, [128, H], F32, 'ctmp')
        cT_r = P.ring(2, [H, 128], F32, 'cT')
        ctx.dma('sp', 'const', bv[:, 0:W], b_in[0:1, 2 * W:3 * W].partition_broadcast(128), writes=[bv])
        ctx.dma('sp', 'const', bv[:, W:2 * W], b_in[0:1, 5 * W:6 * W].partition_broadcast(128), writes=[bv])
        ctx.dma('sp', 'const', bf_[:], b_in[0:1, 6 * W:6 * W + H].partition_broadcast(128), writes=[bf_])
        ctx.dma('sp', 'const', wf[:], wq_v[:, :, 6 * W:6 * W + 64], reads=[r_w], writes=[wf])
        bfT, nbfT = P.sb([H, 1], F32, 'bfT'), P.sb([H, 1], F32, 'nbfT')
        ctx.dma('sp', 'const', bfT[:], b_in[0, 6 * W:6 * W + H].rearrange("(h o) -> h o", o=1), writes=[bfT], allow_slow_non_contiguous=True)
        ctx.op('dve', lambda g: g.tensor_single_scalar(out=nbfT[:], in_=bfT[:], scalar=-1.0, op=ALU.mult), reads=[bfT], writes=[nbfT])
        onesH = P.sb([H, 512], F32, 'onesH')
        ctx.op('dve', lambda g: g.memset(onesH[:], 1.0), writes=[onesH])
        ef_r, cTt_r = P.ring(2, [H, 512], F32, 'ef'), P.ring(2, [H, 512], F32, 'cTt')
        cprev = None
        groupsA = [(0, qT, 0, r_q), (W, kT, 0, r_k), (3 * W, qT, W, r_q), (4 * W, kT, W, r_k)]
        groupsB = [(2 * W, 0), (5 * W, W)]
        for tb in range(S // 512):
            xTb = xTb_r.next()
            ctx.dma('sp', 'xTb_in', xTb[:], xT_v[:, :, tb * 512:(tb + 1) * 512], reads=[r_xT], writes=[xTb], disjoint=False)
            for (col0, dstT, drow, rres) in groupsA:
                for cb in range(W // 512):
                    wb = wblk_r.next()
                    cc0 = col0 + cb * 512
                    ctx.dma('sp', 'w_in', wb[:], wq_v[:, :, cc0:cc0 + 512], reads=[r_w], writes=[wb], disjoint=False)
                    for j in range(4):
                        ps = psA.next()
                        for kc in range(KC):
                            ctx.op('pe', lambda g: g.matmul(ps[:], lhsT=wb[:, kc, j * 128:(j + 1) * 128], rhs=xTb[:, kc, :],
                                                            start=(kc == 0), stop=(kc == KC - 1)),
                                   reads=[wb, xTb], writes=[ps], disjoint=(kc > 0))
                        o = oA.next()
                        bi = (cc0 + j * 128) // 128
                        ctx.op('act', lambda g: g.activation(out=o[:], in_=ps[:], func=AF.Identity,
                                                             bias=binT[:, bi:bi + 1], scale=1.0),
                               reads=[ps, binT], writes=[o])
                        r0 = drow + cb * 512 + j * 128
                        ctx.dma('act', 'qk_out', dstT[r0:r0 + 128, tb * 512:(tb + 1) * 512], o[:], reads=[o], writes=[rres])
            for (col0, dcol) in groupsB:
                for cb in range(W // 512):
                    wb = wblk_r.next()
                    cc0 = col0 + cb * 512
                    ctx.dma('sp', 'w_in', wb[:], wq_v[:, :, cc0:cc0 + 512], reads=[r_w], writes=[wb], disjoint=False)
                    for j in range(4):
                        ps = psA.next()
                        for kc in range(KC):
                            ctx.op('pe', lambda g: g.matmul(ps[:], lhsT=xTb[:, kc, j * 128:(j + 1) * 128], rhs=wb[:, kc, :],
                                                            start=(kc == 0), stop=(kc == KC - 1)),
                                   reads=[wb, xTb], writes=[ps], disjoint=(kc > 0))
                        o = oA.next()
                        bcol = dcol + cb * 512
                        ctx.op('dve', lambda g: g.tensor_tensor(out=o[:], in0=ps[:], in1=bv[:, bcol:bcol + 512], op=ALU.add),
                               reads=[ps, bv], writes=[o])
                        t0 = tb * 512 + j * 128
                        ctx.dma('act', 'v_out', vv[t0:t0 + 128, bcol:bcol + 512], o[:], reads=[o], writes=[r_v])
            psf = psA.next()
            for kc in range(KC):
                ctx.op('pe', lambda g: g.matmul(psf[:H, :], lhsT=wf[:, kc, 0:H], rhs=xTb[:, kc, :], start=(kc == 0), stop=(kc == KC - 1)),
                       reads=[wf, xTb], writes=[psf], disjoint=(kc > 0))
            ef = ef_r.next()
            ctx.op('act', lambda g: g.activation(out=ef[:], in_=psf[:H, :], func=AF.Exp, bias=nbfT[:, 0:1], scale=-1.0), reads=[psf, nbfT], writes=[ef])
            ctx.op('act', lambda g: g.activation(out=ef[:], in_=ef[:], func=AF.Ln, bias=1.0, scale=1.0), reads=[ef], writes=[ef])
            cTt = cTt_r.next()
            if tb == 0:
                ctx.op('dve', lambda g: g.tensor_tensor_scan(out=cTt[:], data0=onesH[:], data1=ef[:], initial=0.0, op0=ALU.mult, op1=ALU.subtract),
                       reads=[onesH, ef], writes=[cTt])
            else:
                ctx.op('dve', lambda g: g.tensor_tensor_scan(out=cTt[:], data0=onesH[:], data1=ef[:], initial=cprev[:, 511:512], op0=ALU.mult, op1=ALU.subtract),
                       reads=[onesH, ef, cprev], writes=[cTt])
            cprev = cTt
            ctx.dma('act', 'c_out', cfxT[:, tb * 512:(tb + 1) * 512], cTt[:], reads=[cTt], writes=[r_c])
        P.close()

        P = Pool_(ctx)
        mfx, msb = P.sb([128, 2048], F32, 'mfx'), P.sb([128, 2048], F32, 'msb')
        ones = P.sb([128, 512], F32, 'ones')
        ctx.dma('sp', 'const', mfx[:], c_mfx[:, :], writes=[mfx])
        ctx.dma('sp', 'const', msb[:], c_msb[:, :], writes=[msb])
        ctx.op('dve', lambda g: g.memset(ones[:], 1.0), writes=[ones])
        NQ = S // 128
        cq = P.sb([128, NQ, H], F32, 'cq')
        for t_ in range(NQ):
            ctx.dma('sp', 'att_in', cq[:, t_, :], cfxT[:, t_ * 128:(t_ + 1) * 128].rearrange("h p -> p h"), reads=[r_c], writes=[cq],
                    disjoint=(t_ > 0), allow_slow_non_contiguous=True)
        qh_r, kh_r = P.ring(2, [128, S], BF16, 'qh'), P.ring(2, [128, S], BF16, 'kh')
        vh_r = P.ring(2, [128, NQ, 128], BF16, 'vh')
        ck_r = P.ring(2, [128, S], F32, 'ck')
        oTh_r = P.ring(2, [128, S], BF16, 'oTh')
        ps_s = P.ring(4, [128, 512], F32, 'ps_s', psum=True)
        ps_t = P.ring(2, [128, 1024], BF16, 'ps_t', psum=True)
        ps_o = P.ring(2, [128, 512], F32, 'ps_o', psum=True)
        t1_r, t2_r, t3_r = P.ring(5, [128, 512], F32, 't1'), P.ring(4, [128, 512], F32, 't2'), P.ring(2, [128, 512], F32, 't3')
        p_r = P.ring(5, [128, 512], BF16, 'p')
        pT_r = P.ring(3, [128, 512], BF16, 'pT')
        rs_r = P.ring(6, [128, 16], F32, 'rs')
        sm_r = P.ring(16, [128, 2], F32, 'sm')
        on_r = P.ring(3, [128, 128], BF16, 'on')
        for br in range(2):
            for h in range(H):
                row0 = br * W + h * 128
                qh, kh, vh, oTh = qh_r.next(), kh_r.next(), vh_r.next(), oTh_r.next()
                ctx.dma('sp', 'att_in', qh[:], qT[row0:row0 + 128, :], reads=[r_q], writes=[qh], disjoint=False)
                ctx.dma('sp', 'att_in', kh[:], kT[row0:row0 + 128, :], reads=[r_k], writes=[kh], disjoint=False)
                ctx.dma('sp', 'att_in', vh[:], vv[:, row0:row0 + 128].rearrange("(t p) d -> p t d", p=128),
                        reads=[r_v], writes=[vh], disjoint=False)
                if br == 1:
                    ck = ck_r.next()
                    ctx.dma('sp', 'att_in', ck[:], cfxT[h:h + 1, :].partition_broadcast(128), reads=[r_c], writes=[ck], disjoint=False)
                blocks = []
                for i in range(NQ):
                    dblk = (i * 128) // 512
                    kbs = list(range(dblk + 1)) if br == 1 else list(range(dblk, -1, -1))
                    for n_, kb in enumerate(kbs):
                        blocks.append(dict(i=i, kb=kb, dblk=dblk, first=(n_ == 0), last=(n_ == len(kbs) - 1)))
                state = {}

                def stage_a1(bk):
                    i, kb, dblk = bk['i'], bk['kb'], bk['dblk']
                    im = i % 4
                    diag = (kb == dblk)
                    if bk['first']:
                        state[i] = dict(pso=ps_o.next(), rs=rs_r.next(), car=None)
                    pss = ps_s.next()
                    bk['pss'] = pss
                    ctx.op('pe', lambda g: g.matmul(pss[:], lhsT=qh[:, i * 128:(i + 1) * 128], rhs=kh[:, kb * 512:(kb + 1) * 512],
                                                    start=True, stop=True), reads=[qh, kh], writes=[pss])
                    t1 = t1_r.next()
                    bk['t1'] = t1
                    if br == 1:
                        ctx.op('dve', lambda g: g.scalar_tensor_tensor(out=t1[:], in0=pss[:], scalar=scale, in1=ck[:, kb * 512:(kb + 1) * 512],
                                                                       op0=ALU.mult, op1=ALU.subtract), reads=[pss, ck], writes=[t1])
                        if diag:
                            ctx.op('pool', lambda g: g.tensor_tensor(out=t1[:], in0=t1[:], in1=mfx[:, im * 512:(im + 1) * 512], op=ALU.add),
                                   reads=[t1, mfx], writes=[t1])
                    else:
                        ctx.op('act', lambda g: g.activation(out=t1[:], in_=pss[:], func=AF.Exp, scale=-scale), reads=[pss], writes=[t1])
                        ctx.op('act', lambda g: g.activation(out=t1[:], in_=t1[:], func=AF.Ln, bias=1.0, scale=1.0), reads=[t1], writes=[t1])

                def stage_a2(bk):
                    if br == 1:
                        return
                    i, kb, dblk = bk['i'], bk['kb'], bk['dblk']
                    im = i % 4
                    diag = (kb == dblk)
                    st = state[i]
                    pss, t1 = bk['pss'], bk['t1']
                    t2, t3 = t2_r.next(), t3_r.next()
                    bk['t2'] = t2
                    ctx.op('dve', lambda g: g.scalar_tensor_tensor(out=t2[:], in0=pss[:], scalar=-scale, in1=t1[:],
                                                                   op0=ALU.mult, op1=ALU.subtract), reads=[pss, t1], writes=[t2])
                    if diag:
                        ctx.op('pool', lambda g: g.tensor_tensor(out=t2[:], in0=t2[:], in1=msb[:, im * 512:(im + 1) * 512], op=ALU.mult),
                               reads=[t2, msb], writes=[t2])
                    ctx.op('dve', lambda g: g.tensor_tensor_scan(out=t3[:], data0=ones[:], data1=t2[:], initial=0.0,
                                                                 op0=ALU.mult, op1=ALU.add), reads=[ones, t2], writes=[t3])
                    sm = sm_r.next()
                    car = st['car']
                    if car is None:
                        ctx.op('dve', lambda g: g.tensor_copy(out=sm[:, 0:1], in_=t3[:, 511:512]), reads=[t3], writes=[sm])
                    else:
                        ctx.op('dve', lambda g: g.tensor_tensor(out=sm[:, 0:1], in0=t3[:, 511:512], in1=car[:, 0:1], op=ALU.add),
                               reads=[t3, car], writes=[sm])
                    st['car'] = sm
                    bk['sm'] = sm
                    ctx.op('dve', lambda g: g.scalar_tensor_tensor(out=t2[:], in0=t3[:], scalar=-1.0, in1=t1[:],
                                                                   op0=ALU.mult, op1=ALU.subtract), reads=[t3, t1], writes=[t2])

                def stage_a3(bk):
                    i, kb, dblk = bk['i'], bk['kb'], bk['dblk']
                    im = i % 4
                    diag = (kb == dblk)
                    st = state[i]
                    rs = st['rs']
                    p = p_r.next()
                    bk['p'] = p
                    if br == 1:
                        t1 = bk['t1']
                        ctx.op('act', lambda g: g.activation(out=p[:], in_=t1[:], func=AF.Exp, bias=cq[:, i, h:h + 1], scale=1.0,
                                                             accum_out=rs[:, kb:kb + 1]), reads=[t1, cq], writes=[p, rs], disjoint=True)
                    else:
                        t2, sm = bk['t2'], bk['sm']
                        if diag:
                            ctx.op('act', lambda g: g.activation(out=t2[:], in_=t2[:], func=AF.Exp, bias=sm[:, 0:1], scale=1.0),
                                   reads=[t2, sm], writes=[t2])
                            ctx.op('pool', lambda g: g.tensor_tensor(out=p[:], in0=t2[:], in1=msb[:, im * 512:(im + 1) * 512], op=ALU.mult),
                                   reads=[t2, msb], writes=[p])
                        else:
                            ctx.op('act', lambda g: g.activation(out=p[:], in_=t2[:], func=AF.Exp, bias=sm[:, 0:1], scale=1.0),
                                   reads=[t2, sm], writes=[p])

                def stage_b(bk):
                    i, kb, dblk = bk['i'], bk['kb'], bk['dblk']
                    im = i % 4
                    diag = (kb == dblk)
                    nj = (im + 1) if diag else 4
                    st = state[i]
                    pso, rs, p = st['pso'], st['rs'], bk['p']
                    pst_ = ps_t.next()
                    for j in range(nj):
                        ctx.op('pe', lambda g: g.transpose(out=pst_[:, j * 128:(j + 1) * 128], in_=p[:, j * 128:(j + 1) * 128], identity=identb[:]),
                               reads=[p, identb], writes=[pst_], disjoint=(j > 0))
                    pT = pT_r.next()
                    if br == 1:
                        ctx.op('dve', lambda g: g.tensor_copy(out=pT[:, :nj * 128], in_=pst_[:, :nj * 128]), reads=[pst_], writes=[pT])
                    else:
                        ctx.op('act', lambda g: g.copy(out=pT[:, :nj * 128], in_=pst_[:, :nj * 128]), reads=[pst_], writes=[pT])
                    for j in range(nj):
                        fm = bk['first'] and j == 0
                        lm = bk['last'] and (j == nj - 1)
                        ctx.op('pe', lambda g: g.matmul(pso[:, 0:128], lhsT=pT[:, j * 128:(j + 1) * 128], rhs=vh[:, kb * 4 + j, :],
                                                        start=fm, stop=lm), reads=[pT, vh], writes=[pso], disjoint=(not fm))
                    if not bk['last']:
                        return
                    on = on_r.next()
                    if br == 1:
                        sm = sm_r.next()
                        ctx.op('dve', lambda g: g.reduce_sum(out=sm[:, 0:1], in_=rs[:, 0:dblk + 1], axis=AX.X), reads=[rs], writes=[sm])
                        ctx.op('dve', lambda g: g.reciprocal(out=sm[:, 1:2], in_=sm[:, 0:1]), reads=[sm], writes=[sm])
                        ctx.op('dve', lambda g: g.tensor_single_scalar(out=on[:], in_=pso[:, 0:128], scalar=sm[:, 1:2], op=ALU.mult),
                               reads=[pso, sm], writes=[on])
                    else:
                        ctx.op('dve', lambda g: g.tensor_copy(out=on[:], in_=pso[:, 0:128]), reads=[pso], writes=[on])
                    pst2 = ps_t.next()
                    ctx.op('pe', lambda g: g.transpose(out=pst2[:, 0:128], in_=on[:], identity=identb[:]), reads=[on, identb], writes=[pst2])
                    ctx.op('act', lambda g: g.copy(out=oTh[:, i * 128:(i + 1) * 128], in_=pst2[:, 0:128]), reads=[pst2], writes=[oTh],
                           disjoint=(i > 0))
                    del state[i]

                stages = (stage_a1, stage_a2, stage_a3, stage_b)
                NB_ = len(blocks)
                for n_ in range(NB_ + len(stages) - 1):
                    for si, fn_ in enumerate(stages):
                        m_ = n_ - si
                        if 0 <= m_ < NB_:
                            fn_(blocks[m_])
                ctx.dma('act', 'oT_out', oT[row0:row0 + 128, :], oTh[:], reads=[oTh], writes=[r_oT])
        P.close()

        P = Pool_(ctx)
        TB = 512
        xTb_r = P.ring(1, [128, KC, TB], BF16, 'xTb')
        oTs_r, oTf_r = P.ring(1, [128, W // 128, TB], BF16, 'oTs'), P.ring(1, [128, W // 128, TB], BF16, 'oTf')
        wg1_r, wg2_r = P.ring(2, [128, KC, 256], BF16, 'wg1'), P.ring(2, [128, KC, 256], BF16, 'wg2')
        wb1_r, wb2_r = P.ring(2, [128, W // 128, 256], BF16, 'wb1'), P.ring(2, [128, W // 128, 256], BF16, 'wb2')
        psM = P.ring(8, [128, TB], F32, 'psM', psum=True)
        g1_r, g2_r = P.ring(2, [128, TB], F32, 'g1'), P.ring(2, [128, TB], F32, 'g2')
        mo_r = P.ring(3, [128, TB], BF16, 'mo')
        oT_v = oT.rearrange("(kc p) s -> p kc s", p=128)
        wbs_v, wbf_v = wbs.rearrange("(kc p) n -> p kc n", p=128), wbf.rearrange("(kc p) n -> p kc n", p=128)
        KW = W // 128
        for tb in range(S // TB):
            xTb, oTs, oTf = xTb_r.next(), oTs_r.next(), oTf_r.next()
            sl = slice(tb * TB, (tb + 1) * TB)
            ctx.dma('sp', 'm_in', xTb[:], xT_v[:, :, sl], reads=[r_xT], writes=[xTb], disjoint=False)
            ctx.dma('sp', 'm_in', oTs[:], oT_v[:, 0:KW, sl], reads=[r_oT], writes=[oTs], disjoint=False)
            ctx.dma('sp', 'm_in', oTf[:], oT_v[:, KW:2 * KW, sl], reads=[r_oT], writes=[oTf], disjoint=False)
            for c2 in range(D // 256):
                wg1, wg2, wb1, wb2 = wg1_r.next(), wg2_r.next(), wb1_r.next(), wb2_r.next()
                ctx.dma('sp', 'm_w', wg1[:], wq_v[:, :, c.G0 + c2 * 256:c.G0 + (c2 + 1) * 256], reads=[r_w], writes=[wg1], disjoint=False)
                ctx.dma('sp', 'm_w', wg2[:], wq_v[:, :, c.G0 + D + c2 * 256:c.G0 + D + (c2 + 1) * 256], reads=[r_w], writes=[wg2], disjoint=False)
                ctx.dma('sp', 'm_w', wb1[:], wbs_v[:, :, c2 * 256:(c2 + 1) * 256], reads=[r_w], writes=[wb1], disjoint=False)
                ctx.dma('sp', 'm_w', wb2[:], wbf_v[:, :, c2 * 256:(c2 + 1) * 256], reads=[r_w], writes=[wb2], disjoint=False)
                for jj in range(2):
                    ct_ = c2 * 2 + jj
                    cs = slice(jj * 128, (jj + 1) * 128)
                    pg1, pg2, pb1, pb2 = psM.next(), psM.next(), psM.next(), psM.next()
                    for (ps, wt, act_, nk) in ((pg1, wg1, xTb, KC), (pg2, wg2, xTb, KC), (pb1, wb1, oTs, KW), (pb2, wb2, oTf, KW)):
                        for kc in range(nk):
                            ctx.op('pe', lambda g: g.matmul(ps[:], lhsT=wt[:, kc, cs], rhs=act_[:, kc, :], start=(kc == 0), stop=(kc == nk - 1)),
                                   reads=[wt, act_], writes=[ps], disjoint=(kc > 0))
                    g1, g2 = g1_r.next(), g2_r.next()
                    ctx.op('act', lambda g: g.activation(out=g1[:], in_=pg1[:], func=AF.Sigmoid, bias=bgT[:, ct_:ct_ + 1], scale=1.0),
                           reads=[pg1, bgT], writes=[g1])
                    ctx.op('act', lambda g: g.activation(out=g2[:], in_=pg2[:], func=AF.Sigmoid, bias=bgT[:, D // 128 + ct_:D // 128 + ct_ + 1], scale=1.0),
                           reads=[pg2, bgT], writes=[g2])
                    ctx.op('dve', lambda g: g.tensor_tensor(out=g1[:], in0=g1[:], in1=pb1[:], op=ALU.mult), reads=[g1, pb1], writes=[g1])
                    ctx.op('dve', lambda g: g.tensor_tensor(out=g2[:], in0=g2[:], in1=pb2[:], op=ALU.mult), reads=[g2, pb2], writes=[g2])
                    mo = mo_r.next()
                    ctx.op('pool', lambda g: g.tensor_tensor(out=mo[:], in0=g1[:], in1=g2[:], op=ALU.add), reads=[g1, g2], writes=[mo])
                    ctx.dma('act', 'mT_out', mT[ct_ * 128:(ct_ + 1) * 128, sl], mo[:], reads=[mo], writes=[r_mT])
        P.close()

        P = Pool_(ctx)
        mTb_r = P.ring(2, [128, KC, 512], BF16, 'mTb')
        wo_r = P.ring(2, [128, KC, 512], BF16, 'wo')
        xr_r = P.ring(4, [128, 512], F32, 'xr')
        z_r = P.ring(4, [128, 512], F32, 'z')
        psZ = P.ring(4, [128, 512], F32, 'psZ', psum=True)
        mT_v = mT.rearrange("(kc p) s -> p kc s", p=128)
        wo_v = wo.rearrange("(kc p) n -> p kc n", p=128)
        for tb in range(S // 512):
            mTb = mTb_r.next()
            ctx.dma('sp', 'o_in', mTb[:], mT_v[:, :, tb * 512:(tb + 1) * 512], reads=[r_mT], writes=[mTb], disjoint=False)
            for cb in range(D // 512):
                wob = wo_r.next()
                ctx.dma('sp', 'o_w', wob[:], wo_v[:, :, cb * 512:(cb + 1) * 512], reads=[r_w], writes=[wob], disjoint=False)
                for j in range(4):
                    ps = psZ.next()
                    for kc in range(KC):
                        ctx.op('pe', lambda g: g.matmul(ps[:], lhsT=mTb[:, kc, j * 128:(j + 1) * 128], rhs=wob[:, kc, :],
                                                        start=(kc == 0), stop=(kc == KC - 1)), reads=[mTb, wob], writes=[ps], disjoint=(kc > 0))
                    t0 = tb * 512 + j * 128
                    xr, z = xr_r.next(), z_r.next()
                    ctx.dma('sp', 'o_x', xr[:], xb_rows[t0:t0 + 128, cb * 512:(cb + 1) * 512], writes=[xr], disjoint=False)
                    ctx.op('dve', lambda g: g.scalar_tensor_tensor(out=z[:], in0=xr[:], scalar=c.alpha, in1=ps[:], op0=ALU.mult, op1=ALU.add),
                           reads=[xr, ps], writes=[z])
                    ctx.dma('act', 'z_out', h1[b][t0:t0 + 128, cb * 512:(cb + 1) * 512], z[:], reads=[z], writes=[r_h1])
        P.close()

    def layer_norm(P, zt, gB, bB, st_r, mv_r):
        st, mv = st_r.next(), mv_r.next()
        nchunk = D // 512 if D >= 512 else 1
        cwid = D // nchunk
        for k in range(nchunk):
            ctx.op('dve', lambda g: g.bn_stats(out=st[:, k * 6:(k + 1) * 6], in_=zt[:, k * cwid:(k + 1) * cwid]), reads=[zt], writes=[st], disjoint=(k > 0))
        ctx.op('dve', lambda g: g.bn_aggr(out=mv[:, 0:2], in_=st[:, 0:nchunk * 6]), reads=[st], writes=[mv])
        ctx.op('dve', lambda g: g.tensor_single_scalar(out=mv[:, 3:4], in_=mv[:, 1:2], scalar=c.eps, op=ALU.add), reads=[mv], writes=[mv])
        ctx.op('act', lambda g: g.activation(out=mv[:, 3:4], in_=mv[:, 3:4], func=AF.Sqrt), reads=[mv], writes=[mv])
        ctx.op('dve', lambda g: g.reciprocal(out=mv[:, 2:3], in_=mv[:, 3:4]), reads=[mv], writes=[mv])
        ctx.op('dve', lambda g: g.tensor_scalar(out=zt[:], in0=zt[:], scalar1=mv[:, 0:1], scalar2=mv[:, 2:3], op0=ALU.subtract, op1=ALU.mult),
               reads=[zt, mv], writes=[zt])
        ctx.op('pool', lambda g: g.tensor_tensor(out=zt[:], in0=zt[:], in1=gB[:], op=ALU.mult), reads=[zt, gB], writes=[zt])
        ctx.op('pool', lambda g: g.tensor_tensor(out=zt[:], in0=zt[:], in1=bB[:], op=ALU.add), reads=[zt, bB], writes=[zt])

    NTT = T // 128
    PL = Pool_(ctx)
    lg_all = PL.sb([128, NTT, E], F32, 'lg_all')
    P = Pool_(ctx)
    g1B, b1B = P.sb([128, D], F32, 'g1B'), P.sb([128, D], F32, 'b1B')
    ctx.dma('sp', 'const', g1B[:], ln1_g[0:1, :].partition_broadcast(128), writes=[g1B])
    ctx.dma('sp', 'const', b1B[:], ln1_b[0:1, :].partition_broadcast(128), writes=[b1B])
    wr = P.sb([128, KC, E], F32, 'wr')
    ctx.dma('sp', 'const', wr[:], w_router.rearrange("(kc p) e -> p kc e", p=128), writes=[wr])
    brB = P.sb([128, E], F32, 'brB')
    ctx.dma('sp', 'const', brB[:], b_router[0:1, :].partition_broadcast(128), writes=[brB])
    zt_r = P.ring(2, [128, D], F32, 'zt')
    hbt_r = P.ring(2, [128, D], BF16, 'hbt')
    hT_r = P.ring(2, [128, KC, 128], F32, 'hT')
    st_r, mv_r = P.ring(2, [128, 48], F32, 'st'), P.ring(2, [128, 4], F32, 'mv')
    psT = P.ring(4, [128, 512], F32, 'psT', psum=True)
    psL = P.ring(2, [128, 512], F32, 'psL', psum=True)
    for b in range(BL):
        for tt in range(S // 128):
            zt = zt_r.next()
            rows = slice(tt * 128, (tt + 1) * 128)
            ctx.dma('sp', 'ln_in', zt[:], h1[b][rows, :], reads=[r_h1], writes=[zt], disjoint=False)
            layer_norm(P, zt, g1B, b1B, st_r, mv_r)
            ctx.dma('act', 'h_out', h1[b][rows, :], zt[:], reads=[zt], writes=[r_h1])
            hbt = hbt_r.next()
            ctx.op('act', lambda g: g.copy(out=hbt[:], in_=zt[:]), reads=[zt], writes=[hbt])
            ctx.dma('act', 'h_out', hb[b][rows, :], hbt[:], reads=[hbt], writes=[r_hb])
            hT = hT_r.next()
            for k4 in range(KC // 4):
                ps = psT.next()
                for kk in range(4):
                    kc = k4 * 4 + kk
                    ctx.op('pe', lambda g: g.transpose(out=ps[:, kk * 128:(kk + 1) * 128], in_=zt[:, kc * 128:(kc + 1) * 128], identity=identf[:]),
                           reads=[zt, identf], writes=[ps], disjoint=(kk > 0))
                ctx.op('dve', lambda g: g.tensor_copy(out=hT[:, k4 * 4:(k4 + 1) * 4, :], in_=ps[:, :].rearrange("p (k t) -> p k t", k=4)),
                       reads=[ps], writes=[hT], disjoint=(k4 > 0))
            psl = psL.next()
            for kc in range(KC):
                ctx.op('pe', lambda g: g.matmul(psl[:, 0:E], lhsT=hT[:, kc, :], rhs=wr[:, kc, :], start=(kc == 0), stop=(kc == KC - 1)),
                       reads=[hT, wr], writes=[psl], disjoint=(kc > 0))
            gi = b * (S // 128) + tt
            ctx.op('dve', lambda g: g.tensor_tensor(out=lg_all[:, gi, :], in0=psl[:, 0:E], in1=brB[:], op=ALU.add), reads=[psl, brB], writes=[lg_all], disjoint=True)
    P.close()

    NT = CH // 128
    NS = CAP // 128
    KF = DFF // 128
    nchunks = T // CH
    xe_l = [dscr("s_xe%d" % i, [E * CAP, D], BF16) for i in range(nchunks)]
    ye_l = [dscr("s_ye%d" % i, [E * CAP, D], F32) for i in range(nchunks)]
    r_xe_l, r_ye_l = [R("xe") for _ in range(nchunks)], [R("ye") for _ in range(nchunks)]
    bupT = PL.sb([128, E, 2 * KF], F32, 'bupT')
    ctx.dma('sp', 'const', bupT[:], b_up.rearrange("e (t p) -> p e t", p=128), writes=[bupT], allow_slow_non_contiguous=True)
    gat_l = [PL.sb([128, NT, 4], F32, 'gat') for _ in range(nchunks)]
    sloti_l = [PL.sb([128, NT * 4], I32, 'sloti') for _ in range(nchunks)]
    for ch in range(nchunks):
        b = (ch * CH) // S
        roff = (ch * CH) % S
        gat, sloti, xe_c, r_xe_c = gat_l[ch], sloti_l[ch], xe_l[ch], r_xe_l[ch]
        P = Pool_(ctx)
        ecap = P.sb([128, E], F32, 'ecap')
        ctx.dma('sp', 'const', ecap[:], c_ecap[:, :], writes=[ecap])
        top8 = P.sb([128, NT, 8], F32, 'top8')
        Mall = P.sb([128, NT, E], BF16, 'Mall')
        Mf = P.sb([128, NT, E], F32, 'Mf')
        OH = P.sb([128, 4, NT, E], F32, 'OH')
        posf = P.sb([128, NT, E], F32, 'posf')
        basef = P.sb([128, NT, E], F32, 'basef')
        slotf = P.sb([128, NT, 4], F32, 'slotf')
        tmpE = P.sb([128, NT, E], F32, 'tmpE')
        negm = P.sb([128, NT], F32, 'negm')
        den = P.sb([128, NT], F32, 'den')
        psR = P.ring(2, [128, NT * E], F32, 'psR', psum=True)
        for tt in range(NT):
            gi = ch * NT + tt
            lg = lg_all[:, gi, :]
            ctx.op('dve', lambda g: g.max(out=top8[:, tt, :], in_=lg), reads=[lg_all], writes=[top8], disjoint=(tt > 0))
            ctx.op('dve', lambda g: g.tensor_single_scalar(out=Mf[:, tt, :], in_=lg, scalar=top8[:, tt, 3:4], op=ALU.is_ge),
                   reads=[lg_all, top8], writes=[Mf], disjoint=(tt > 0))
            for k in range(4):
                ctx.op('dve', lambda g: g.tensor_single_scalar(out=OH[:, k, tt, :], in_=lg, scalar=top8[:, tt, k:k + 1], op=ALU.is_equal),
                       reads=[lg_all, top8], writes=[OH], disjoint=(tt > 0 or k > 0))
            ctx.op('dve', lambda g: g.tensor_single_scalar(out=negm[:, tt:tt + 1], in_=top8[:, tt, 0:1], scalar=-1.0, op=ALU.mult),
                   reads=[top8], writes=[negm], disjoint=(tt > 0))
            ctx.op('act', lambda g: g.activation(out=gat[:, tt, :], in_=top8[:, tt, 0:4], func=AF.Exp, bias=negm[:, tt:tt + 1], scale=1.0,
                                                 accum_out=den[:, tt:tt + 1]), reads=[top8, negm], writes=[gat, den], disjoint=(tt > 0))
            ctx.op('dve', lambda g: g.reciprocal(out=den[:, tt:tt + 1], in_=den[:, tt:tt + 1]), reads=[den], writes=[den], disjoint=True)
            ctx.op('dve', lambda g: g.tensor_single_scalar(out=gat[:, tt, :], in_=gat[:, tt, :], scalar=den[:, tt:tt + 1], op=ALU.mult),
                   reads=[gat, den], writes=[gat], disjoint=True)
        ctx.op('dve', lambda g: g.tensor_copy(out=Mall[:], in_=Mf[:]), reads=[Mf], writes=[Mall])
        Mflat = Mall[:, :, :].rearrange("p t e -> p (t e)")
        ps1, ps2 = psR.next(), psR.next()
        ctx.op('pe', lambda g: g.matmul(ps1[:], lhsT=trib[:, 128:256], rhs=Mflat, start=True, stop=True), reads=[trib, Mall], writes=[ps1])
        ctx.op('pe', lambda g: g.matmul(ps2[:], lhsT=trib[:, 256:384], rhs=Mflat, start=True, stop=True), reads=[trib, Mall], writes=[ps2])
        ps2v = ps2[:, :].rearrange("p (t e) -> p t e", e=E)
        ctx.op('dve', lambda g: g.memset(basef[:, 0, :], 0.0), writes=[basef])
        for tt in range(1, NT):
            ctx.op('dve', lambda g: g.tensor_tensor(out=basef[:, tt, :], in0=basef[:, tt - 1, :], in1=ps2v[:, tt - 1, :], op=ALU.add),
                   reads=[basef, ps2], writes=[basef], disjoint=True)
        ctx.op('dve', lambda g: g.tensor_tensor(out=posf[:], in0=basef[:], in1=ps1[:, :].rearrange("p (t e) -> p t e", e=E), op=ALU.add),
               reads=[basef, ps1], writes=[posf])
        ctx.op('dve', lambda g: g.tensor_single_scalar(out=posf[:], in_=posf[:], scalar=float(CAP - 1), op=ALU.min), reads=[posf], writes=[posf])
        for tt in range(NT):
            ctx.op('dve', lambda g: g.tensor_tensor(out=posf[:, tt, :], in0=posf[:, tt, :], in1=ecap[:], op=ALU.add), reads=[posf, ecap], writes=[posf], disjoint=True)
        for k in range(4):
            ctx.op('dve', lambda g: g.tensor_tensor(out=tmpE[:], in0=OH[:, k, :, :], in1=posf[:], op=ALU.mult), reads=[OH, posf], writes=[tmpE])
            ctx.op('dve', lambda g: g.reduce_sum(out=slotf[:, :, k], in_=tmpE[:], axis=AX.X), reads=[tmpE], writes=[slotf], disjoint=(k > 0))
        ctx.op('dve', lambda g: g.tensor_copy(out=sloti[:], in_=slotf[:, :, :].rearrange("p t k -> p (t k)")), reads=[slotf], writes=[sloti])
        hbt_r = P.ring(3, [128, D], BF16, 'hbt')
        for tt in range(NT):
            hbt = hbt_r.next()
            rows = slice(roff + tt * 128, roff + (tt + 1) * 128)
            ctx.dma('sp', 'd_in', hbt[:], hb[b][rows, :], reads=[r_hb], writes=[hbt], disjoint=False)
            for k in range(4):
                col = tt * 4 + k
                ctx.dma('pool', 'scat', None, None, reads=[hbt, sloti], writes=[r_xe_c],
                        fn=lambda g: g.indirect_dma_start(out=xe_c[:, :], out_offset=bass.IndirectOffsetOnAxis(ap=sloti[:, col:col + 1], axis=0),
                                                          in_=hbt[:], in_offset=None))
        P.close()
    P = Pool_(ctx)
    xs_r = P.ring(1, [128, D], BF16, 'xs')
    xeT_l = [P.sb([128, KC, CAP], BF16, 'xeT') for _ in range(nchunks)]
    aT_l = [P.sb([128, KF, CAP], BF16, 'aT') for _ in range(nchunks)]
    stg_r = P.ring(3, [128, 4096], F32, 'stg')
    wb_r = P.ring(4, [128, 4096], BF16, 'wb')
    bd_r = P.ring(2, [128, 256], F32, 'bd')
    y_r = P.ring(3, [128, 256], F32, 'y')
    e1_r, e2_r, e3_r = P.ring(2, [128, CAP], F32, 'e1'), P.ring(2, [128, CAP], F32, 'e2'), P.ring(2, [128, CAP], F32, 'e3')
    psX = P.ring(2, [128, 1024], BF16, 'psX', psum=True)
    psU = P.ring(4, [128, 512], F32, 'psU', psum=True)
    psD = P.ring(2, [128, 512], F32, 'psD', psum=True)
    assert KC * 128 == 4096 or KC * 128 <= 4096
    ccnt = [0]

    def load_cast(src_ap, nk, ncol):
        st_, wb_ = stg_r.next(), wb_r.next()
        sv = st_[:, 0:nk * ncol].rearrange("p (k n) -> p k n", n=ncol)
        wv = wb_[:, 0:nk * ncol].rearrange("p (k n) -> p k n", n=ncol)
        ctx.dma('sp', 'e_w', sv, src_ap, writes=[st_], disjoint=False)
        eng = ('pool', 'act')[ccnt[0] % 2]
        ccnt[0] += 1
        if eng == 'act':
            ctx.op('act', lambda g: g.copy(out=wb_[:, 0:nk * ncol], in_=st_[:, 0:nk * ncol]), reads=[st_], writes=[wb_])
        else:
            ctx.op('pool', lambda g: g.tensor_copy(out=wb_[:, 0:nk * ncol], in_=st_[:, 0:nk * ncol]), reads=[st_], writes=[wb_])
        return wb_, wv

    for e_ in range(E):
        wu_e = w_up[e_ * D:(e_ + 1) * D, :].rearrange("(kc p) n -> p kc n", p=128)
        wd_e = w_down[e_ * DFF:(e_ + 1) * DFF, :].rearrange("(kc p) n -> p kc n", p=128)
        for ch in range(nchunks):
            xT_e = xeT_l[ch]
            for st_i in range(NS):
                xs = xs_r.next()
                r0 = e_ * CAP + st_i * 128
                ctx.dma('sp', 'e_in', xs[:], xe_l[ch][r0:r0 + 128, :], reads=[r_xe_l[ch]], writes=[xs], disjoint=False)
                for k4 in range(KC // 4):
                    ps = psX.next()
                    for kk in range(4):
                        kc = k4 * 4 + kk
                        ctx.op('pe', lambda g: g.transpose(out=ps[:, kk * 128:(kk + 1) * 128], in_=xs[:, kc * 128:(kc + 1) * 128], identity=identb[:]),
                               reads=[xs, identb], writes=[ps], disjoint=(kk > 0))
                    dst = xT_e[:, k4 * 4:(k4 + 1) * 4, st_i * 128:(st_i + 1) * 128]
                    src = ps[:, 0:512].rearrange("p (k t) -> p k t", k=4)
                    ctx.op('dve', lambda g: g.tensor_copy(out=dst, in_=src), reads=[ps], writes=[xT_e], disjoint=not (st_i == 0 and k4 == 0))
        for j in range(KF):
            wgR, wg = load_cast(wu_e[:, :, j * 128:(j + 1) * 128], KC, 128)
            wuR, wu = load_cast(wu_e[:, :, DFF + j * 128:DFF + (j + 1) * 128], KC, 128)
            for ch in range(nchunks):
                xT_e, aT = xeT_l[ch], aT_l[ch]
                pg, pu = psU.next(), psU.next()
                for (ps, wR, wt) in ((pg, wgR, wg), (pu, wuR, wu)):
                    for kc in range(KC):
                        ctx.op('pe', lambda g: g.matmul(ps[:, :CAP], lhsT=wt[:, kc, :], rhs=xT_e[:, kc, :], start=(kc == 0), stop=(kc == KC - 1)),
                               reads=[wR, xT_e], writes=[ps], disjoint=(kc > 0))
                e1, e2, e3 = e1_r.next(), e2_r.next(), e3_r.next()
                ctx.op('dve', lambda g: g.tensor_scalar(out=e1[:], in0=pg[:, :CAP], scalar1=bupT[:, e_, j:j + 1], scalar2=7.0, op0=ALU.add, op1=ALU.min),
                       reads=[pg, bupT], writes=[e1])
                ctx.op('act', lambda g: g.activation(out=e2[:], in_=e1[:], func=AF.Sigmoid, scale=1.702), reads=[e1], writes=[e2])
                ctx.op('dve', lambda g: g.tensor_scalar(out=e3[:], in0=pu[:, :CAP], scalar1=bupT[:, e_, KF + j:KF + j + 1], scalar2=7.0, op0=ALU.add, op1=ALU.min),
                       reads=[pu, bupT], writes=[e3])
                ctx.op('pool', lambda g: g.tensor_scalar(out=e3[:], in0=e3[:], scalar1=-7.0, scalar2=1.0, op0=ALU.max, op1=ALU.add), reads=[e3], writes=[e3])
                ctx.op('pool', lambda g: g.tensor_tensor(out=e1[:], in0=e1[:], in1=e2[:], op=ALU.mult), reads=[e1, e2], writes=[e1])
                ctx.op('dve', lambda g: g.tensor_tensor(out=aT[:, j, :], in0=e1[:], in1=e3[:], op=ALU.mult), reads=[e1, e3], writes=[aT], disjoint=(j > 0))
        for cb in range(D // 256):
            wdR, wd = load_cast(wd_e[:, :, cb * 256:(cb + 1) * 256], KF, 256)
            bd = bd_r.next()
            ctx.dma('sp', 'e_w', bd[:], b_down[e_:e_ + 1, cb * 256:(cb + 1) * 256].partition_broadcast(128), writes=[bd], disjoint=False)
            for ch in range(nchunks):
                aT = aT_l[ch]
                for st_i in range(NS):
                    ps = psD.next()
                    for kc in range(KF):
                        ctx.op('pe', lambda g: g.matmul(ps[:, 0:256], lhsT=aT[:, kc, st_i * 128:(st_i + 1) * 128], rhs=wd[:, kc, :], start=(kc == 0), stop=(kc == KF - 1)),
                               reads=[aT, wdR], writes=[ps], disjoint=(kc > 0))
                    y = y_r.next()
                    ctx.op('dve', lambda g: g.tensor_tensor(out=y[:], in0=ps[:, 0:256], in1=bd[:], op=ALU.add), reads=[ps, bd], writes=[y])
                    r0 = e_ * CAP + st_i * 128
                    ctx.dma('act', 'y_out', ye_l[ch][r0:r0 + 128, cb * 256:(cb + 1) * 256], y[:], reads=[y], writes=[r_ye_l[ch]])
    P.close()
    P = Pool_(ctx)
    g2B, b2B = P.sb([128, D], F32, 'g2B'), P.sb([128, D], F32, 'b2B')
    ctx.dma('sp', 'const', g2B[:], ln2_g[0:1, :].partition_broadcast(128), writes=[g2B])
    ctx.dma('sp', 'const', b2B[:], ln2_b[0:1, :].partition_broadcast(128), writes=[b2B])
    yg_r = P.ring(3, [128, D], F32, 'yg')
    acc_r = P.ring(2, [128, D], F32, 'acc')
    hr_r = P.ring(2, [128, D], F32, 'hr')
    st_r, mv_r = P.ring(2, [128, 48], F32, 'st'), P.ring(2, [128, 4], F32, 'mv')
    for ch in range(nchunks):
        b = (ch * CH) // S
        roff = (ch * CH) % S
        gat, sloti, ye_c, r_ye_c = gat_l[ch], sloti_l[ch], ye_l[ch], r_ye_l[ch]
        for tt in range(NT):
            acc = acc_r.next()
            for k in range(4):
                yg = yg_r.next()
                col = tt * 4 + k
                ctx.dma('pool', 'gath', None, None, reads=[r_ye_c, sloti], writes=[yg], disjoint=False,
                        fn=lambda g: g.indirect_dma_start(out=yg[:], out_offset=None, in_=ye_c[:, :],
                                                          in_offset=bass.IndirectOffsetOnAxis(ap=sloti[:, col:col + 1], axis=0)))
                if k == 0:
                    ctx.op('dve', lambda g: g.tensor_single_scalar(out=acc[:], in_=yg[:], scalar=gat[:, tt, 0:1], op=ALU.mult),
                           reads=[yg, gat], writes=[acc])
                else:
                    ctx.op('dve', lambda g: g.scalar_tensor_tensor(out=acc[:], in0=yg[:], scalar=gat[:, tt, k:k + 1], in1=acc[:], op0=ALU.mult, op1=ALU.add),
                           reads=[yg, gat, acc], writes=[acc])
            hr = hr_r.next()
            rows = slice(roff + tt * 128, roff + (tt + 1) * 128)
            ctx.dma('sp', 'c_in', hr[:], h1[b][rows, :], reads=[r_h1], writes=[hr], disjoint=False)
            ctx.op('dve', lambda g: g.scalar_tensor_tensor(out=acc[:], in0=hr[:], scalar=c.alpha, in1=acc[:], op0=ALU.mult, op1=ALU.add),
                   reads=[hr, acc], writes=[acc])
            layer_norm(P, acc, g2B, b2B, st_r, mv_r)
            orow = ch * CH + tt * 128
            ctx.dma('act', 'out', out[orow:orow + 128, :], acc[:], reads=[acc], writes=[r_out])
    P.close()
    PL.close()
    P0.close()
    ctx.barrier()
    es.close()
    return nc, ctx


def host_consts(cfg):
    E, CAP = cfg.E, cfg.CAP
    cs = {}
    cs["c_identb"] = np.eye(128, dtype=np.float32).astype(ml_dtypes.bfloat16)
    cs["c_identf"] = np.eye(128, dtype=np.float32)
    q = np.arange(128)[:, None]
    k = np.arange(512)[None, :]
    mf = np.zeros((128, 4 * 512), np.float32)
    ms = np.zeros((128, 4 * 512), np.float32)
    for im in range(4):
        qpos = im * 128 + q
        mf[:, im * 512:(im + 1) * 512] = np.where(k <= qpos, 0.0, -30000.0)
        ms[:, im * 512:(im + 1) * 512] = np.where(k < qpos, 1.0, 0.0)
    cs["c_mfx"], cs["c_msb"] = mf, ms
    j = np.arange(128)[:, None]
    i = np.arange(128)[None, :]
    cs["c_tri"] = np.concatenate([(j <= i), (j < i), np.ones((128, 128), bool)], axis=1).astype(np.float32)
    cs["c_ecap"] = np.tile((np.arange(E) * CAP).astype(np.float32)[None, :], (128, 1))
    return cs


def run(cfg, ncores, inputs, trace=False, debug=False):
    nc, ctx = build(cfg, ncores, debug)
    c = cfg
    T, D, E, DFF = c.T, c.D, c.E, c.DFF
    cs = host_consts(cfg)
    f = lambda a: np.ascontiguousarray(np.asarray(a, dtype=np.float32))
    xs = f(inputs["x"]).reshape(ncores, T, D)
    shared = {
        "w_in": f(inputs["w_in"])[0], "b_in": f(inputs["b_in"]).reshape(1, -1),
        "w_branch_sb": f(inputs["w_branch_sb"])[0], "w_branch_fx": f(inputs["w_branch_fx"])[0],
        "w_out": f(inputs["w_out"])[0], "ln1_g": f(inputs["ln1_g"]).reshape(1, -1), "ln1_b": f(inputs["ln1_b"]).reshape(1, -1),
        "w_router": f(inputs["w_router"])[0], "b_router": f(inputs["b_router"]).reshape(1, -1),
        "w_up": f(inputs["w_up"]).reshape(E * D, 2 * DFF), "b_up": f(inputs["b_up"]).reshape(E, 2 * DFF),
        "w_down": f(inputs["w_down"]).reshape(E * DFF, D), "b_down": f(inputs["b_down"]).reshape(E, D),
        "ln2_g": f(inputs["ln2_g"]).reshape(1, -1), "ln2_b": f(inputs["ln2_b"]).reshape(1, -1),
    }
    shared.update(cs)
    in_maps = [dict(shared, x=xs[i]) for i in range(ncores)]
    res = run_bass_kernel_spmd(nc, in_maps, core_ids=list(range(ncores)), **({"trace": True} if trace else {}))
    outs = [np.asarray(r["out"], dtype=np.float32) for r in res.results]
    return np.concatenate(outs, axis=0), res


def kernel(**inputs):
    B, S, D = inputs["x"].shape
    cfg = Cfg(D=D, S=S, BL=B // NCORES)
    o, _ = run(cfg, NCORES, inputs)
    return o.reshape(B, S, D)
```

```python
import numpy as np
import ml_dtypes
from contextlib import ExitStack
import concourse.bass as bass
import concourse.mybir as mybir
from concourse.bass_utils import run_bass_kernel_spmd

F32, BF16, I32 = mybir.dt.float32, mybir.dt.bfloat16, mybir.dt.int32
AF = mybir.ActivationFunctionType
ALU = mybir.AluOpType
AX = mybir.AxisListType

NCORES = 4
DEBUG_NAMES = ('s_dbg', 's_dbg2', 's_dbg3', 's_xT', 's_qT', 's_kT', 's_v', 's_cfx', 's_cfxT', 's_oT', 's_mT', 's_h1_0', 's_hb_0', 's_xe', 's_ye', 's_wq')


class Cfg:
    def __init__(self, D=4096, S=4096, BL=4, H=16, E=32, CH=2048, CAP=384):
        self.D, self.S, self.BL, self.H, self.E, self.CH, self.CAP = D, S, BL, H, E, CH, CAP
        self.W = H * 128
        self.DFF = D // 2
        self.PROJ = 6 * self.W + H + 2 * D
        self.KC = D // 128
        self.T = BL * S
        self.G0 = 6 * self.W + H
        self.alpha = 2.0 ** 0.25
        self.eps = 1e-5


class Res:
    def __init__(self, name, obj=None):
        self.name, self.obj, self.w, self.r = name, obj, {}, {}
        self.dsem = None

    def __getitem__(self, idx):
        return self.obj[idx]


class Ctx:
    def __init__(self, nc, es):
        self.nc, self.es = nc, es
        self.eng = {'pe': nc.tensor, 'act': nc.scalar, 'dve': nc.vector, 'pool': nc.gpsimd, 'sp': nc.sync}
        self.psem = {k: es.enter_context(nc.semaphore('p_' + k)) for k in self.eng}
        self.pcnt = {k: 0 for k in self.eng}
        self.waited = {k: {} for k in self.eng}
        self.dsem = {}
        self.sem_all, self.sem_free = [], []
        self.nins = 0

    def borrow_sem(self):
        if self.sem_free:
            return self.sem_free.pop()
        key = 'd_%d' % len(self.sem_all)
        ent = [self.es.enter_context(self.nc.semaphore(key)), 0, key]
        self.sem_all.append(ent)
        return ent

    def _wait(self, e, events, own_ok=True):
        for name, (sem, val) in events.items():
            if own_ok and name == 'p_' + e and e in ('pe', 'sp'):
                continue
            if self.waited[e].get(name, 0) >= val:
                continue
            self.eng[e].wait_ge(sem, val)
            self.waited[e][name] = val

    def _deps(self, e, reads, writes, disjoint, own_ok=True):
        for r in reads:
            self._wait(e, r.w, own_ok)
        for w in writes:
            self._wait(e, w.r, own_ok)
            self._wait(e, w.w, own_ok)

    def _mark(self, ev, reads, writes, disjoint):
        for r in reads:
            r.r[ev[0]] = ev[1]
        for w in writes:
            if not disjoint:
                w.w = {}
                w.r = {}
            w.w[ev[0]] = ev[1]

    def op(self, e, fn, reads=(), writes=(), disjoint=False):
        self._deps(e, reads, writes, disjoint)
        ins = fn(self.eng[e])
        self.pcnt[e] += 1
        ins.then_inc(self.psem[e], 1)
        self._mark(('p_' + e, (self.psem[e], self.pcnt[e])), reads, writes, disjoint)
        self.nins += 1
        return ins

    def dma(self, q, stream, out, in_, reads=(), writes=(), disjoint=True, fn=None, **kw):
        side = [r_ for r_ in list(writes) + list(reads) if r_.obj is not None]
        assert side, stream
        owner = side[0]
        if owner.dsem is None:
            owner.dsem = self.borrow_sem()
        ent = owner.dsem
        key = ent[2]
        self._deps(q, reads, writes, disjoint, own_ok=False)
        if fn is None:
            ins = self.eng[q].dma_start(out=out, in_=in_, **kw)
        else:
            ins = fn(self.eng[q])
        ent[1] += 16
        ins.then_inc(ent[0], 16)
        self._mark((key, (ent[0], ent[1])), reads, writes, disjoint)
        self.nins += 1
        return ins

    def barrier(self):
        evs = {'p_' + k: (self.psem[k], self.pcnt[k]) for k in self.eng if self.pcnt[k] > 0}
        for ent in self.sem_all:
            if ent[1] > 0:
                evs[ent[2]] = (ent[0], ent[1])
        for e in self.eng:
            self._wait(e, evs)


class Pool_:
    def __init__(self, ctx):
        self.ctx, self.es = ctx, ExitStack()
        self.n = 0
        self.res = []

    def sb(self, shape, dt, name='t'):
        self.n += 1
        t = self.es.enter_context(self.ctx.nc.sbuf_tensor('%s_%d_%d' % (name, id(self) % 9973, self.n), list(shape), dt))
        r_ = Res(name, t)
        self.res.append(r_)
        return r_

    def ps(self, shape, dt, name='ps'):
        self.n += 1
        t = self.es.enter_context(self.ctx.nc.psum_tensor('%s_%d_%d' % (name, id(self) % 9973, self.n), list(shape), dt))
        return Res(name, t)

    def ring(self, n, shape, dt, name='r', psum=False):
        return Ring([(self.ps if psum else self.sb)(shape, dt, name) for _ in range(n)])

    def close(self):
        self.ctx.barrier()
        for r_ in self.res:
            if r_.dsem is not None:
                self.ctx.sem_free.append(r_.dsem)
                r_.dsem = None
        self.es.close()


class Ring:
    def __init__(self, items):
        self.items, self.i = items, -1

    def next(self):
        self.i = (self.i + 1) % len(self.items)
        return self.items[self.i]


def build(cfg, ncores, debug=False):
    c = cfg
    D, S, BL, H, E, W, DFF, KC, T, CAP, CH = c.D, c.S, c.BL, c.H, c.E, c.W, c.DFF, c.KC, c.T, c.CAP, c.CH
    nc = bass.Bass("TRN2", target_bir_lowering=False)
    dt_in = lambda n, s, d=F32: nc.dram_tensor(n, list(s), d, kind="ExternalInput").ap()
    x = dt_in("x", [T, D])
    w_in = dt_in("w_in", [D, c.PROJ])
    b_in = dt_in("b_in", [1, c.PROJ])
    w_bs = dt_in("w_branch_sb", [W, D])
    w_bf = dt_in("w_branch_fx", [W, D])
    w_out = dt_in("w_out", [D, D])
    ln1_g, ln1_b = dt_in("ln1_g", [1, D]), dt_in("ln1_b", [1, D])
    w_router, b_router = dt_in("w_router", [D, E]), dt_in("b_router", [1, E])
    w_up, b_up = dt_in("w_up", [E * D, 2 * DFF]), dt_in("b_up", [E, 2 * DFF])
    w_down, b_down = dt_in("w_down", [E * DFF, D]), dt_in("b_down", [E, D])
    ln2_g, ln2_b = dt_in("ln2_g", [1, D]), dt_in("ln2_b", [1, D])
    c_identb = dt_in("c_identb", [128, 128], BF16)
    c_identf = dt_in("c_identf", [128, 128])
    c_mfx = dt_in("c_mfx", [128, 4 * 512])
    c_msb = dt_in("c_msb", [128, 4 * 512])
    c_tri = dt_in("c_tri", [128, 3 * 128])
    c_ecap = dt_in("c_ecap", [128, E])
    out = nc.dram_tensor("out", [T, D], F32, kind="ExternalOutput").ap()

    dscr = lambda n, s, d: (nc.dram_tensor(n, list(s), d, kind='ExternalOutput').ap() if (debug and n in DEBUG_NAMES) else nc.dram_tensor(n, list(s), d).ap())
    wq = dscr("s_wq", [D, c.PROJ], BF16)
    wbs, wbf, wo = dscr("s_wbs", [W, D], BF16), dscr("s_wbf", [W, D], BF16), dscr("s_wo", [D, D], BF16)
    xT = dscr("s_xT", [D, S], BF16)
    qT, kT = dscr("s_qT", [2 * W, S], BF16), dscr("s_kT", [2 * W, S], BF16)
    vv = dscr("s_v", [S, 2 * W], BF16)
    cfx, cfxT = dscr("s_cfx", [S, H], F32), dscr("s_cfxT", [H, S], F32)
    oT = dscr("s_oT", [2 * W, S], BF16)
    dbg = dscr("s_dbg", [S, 3 * H], F32)
    dbg2 = dscr("s_dbg2", [128, 5 * 512], F32)
    dbg3 = dscr("s_dbg3", [128, 3 * 512], BF16)
    mT = dscr("s_mT", [D, S], BF16)
    h1 = [dscr("s_h1_%d" % b, [S, D], F32) for b in range(BL)]
    hb = [dscr("s_hb_%d" % b, [S, D], BF16) for b in range(BL)]

    es = ExitStack()
    ctx = Ctx(nc, es)
    R = lambda n: Res(n)
    r_w = R("weights")
    r_xT, r_q, r_k, r_v, r_c, r_oT, r_mT = R("xT"), R("qT"), R("kT"), R("v"), R("c"), R("oT"), R("mT")
    r_h1, r_hb, r_xe, r_ye, r_out = R("h1"), R("hb"), R("xe"), R("ye"), R("out")
    scale = 128 ** -0.5

    P0 = Pool_(ctx)
    identb, identf = P0.sb([128, 128], BF16, 'identb'), P0.sb([128, 128], F32, 'identf')
    tri = P0.sb([128, 384], F32, 'tri')
    trib = P0.sb([128, 384], BF16, 'trib')
    ctx.dma('sp', 'const', identb[:], c_identb[:, :], writes=[identb])
    ctx.dma('sp', 'const', identf[:], c_identf[:, :], writes=[identf])
    ctx.dma('sp', 'const', tri[:], c_tri[:, :], writes=[tri])
    ctx.op('dve', lambda e: e.tensor_copy(out=trib[:], in_=tri[:]), reads=[tri], writes=[trib])

    def cast_matrix(P, src, dst, rows, cols, rings, cnt):
        src_v = src.rearrange("(r p) c -> r p c", p=128)
        dst_v = dst.rearrange("(r p) c -> r p c", p=128)
        cw = min(cols, 4096)
        for r in range(rows // 128):
            for c0 in range(0, cols, cw):
                cc = min(cw, cols - c0)
                a, b_ = rings[0].next(), rings[1].next()
                ctx.dma('sp', 'cast_in', a[:, :cc], src_v[r, :, c0:c0 + cc], writes=[a], disjoint=False)
                e = ('dve', 'act')[cnt[0] % 2]
                cnt[0] += 1
                if e == 'act':
                    ctx.op(e, lambda g: g.copy(out=b_[:, :cc], in_=a[:, :cc]), reads=[a], writes=[b_])
                else:
                    ctx.op(e, lambda g: g.tensor_copy(out=b_[:, :cc], in_=a[:, :cc]), reads=[a], writes=[b_])
                ctx.dma('act', 'cast_out', dst_v[r, :, c0:c0 + cc], b_[:, :cc], reads=[b_], writes=[r_w])

    P = Pool_(ctx)
    rings = (P.ring(3, [128, 4096], F32, 'ci'), P.ring(3, [128, 4096], BF16, 'co'))
    cnt = [0]
    cast_matrix(P, w_in, wq, D, c.PROJ, rings, cnt)
    cast_matrix(P, w_bs, wbs, W, D, rings, cnt)
    cast_matrix(P, w_bf, wbf, W, D, rings, cnt)
    cast_matrix(P, w_out, wo, D, D, rings, cnt)
    P.close()

    wq_v = wq.rearrange("(kc p) n -> p kc n", p=128)
    xT_v = xT.rearrange("(kc p) s -> p kc s", p=128)

    NB = (6 * W) // 128
    NG = (2 * D) // 128
    binT = P0.sb([128, NB], F32, 'binT')
    bgT = P0.sb([128, NG], F32, 'bgT')
    ctx.dma('sp', 'const', binT[:], b_in[0, 0:6 * W].rearrange("(t p) -> p t", p=128), writes=[binT],
            allow_slow_non_contiguous=True)
    ctx.dma('sp', 'const', bgT[:], b_in[0, c.G0:c.G0 + 2 * D].rearrange("(t p) -> p t", p=128), writes=[bgT],
            allow_slow_non_contiguous=True)

    for b in range(BL):
        xb_rows = x[b * S:(b + 1) * S, :]
        P = Pool_(ctx)
        xin = P.ring(2, [128, D], F32, 'xin')
        xbf = P.ring(2, [128, D], BF16, 'xbf')
        xTt = P.ring(2, [128, KC, 512], BF16, 'xTt')
        pst = P.ring(4, [128, 1024], BF16, 'pst', psum=True)
        for tb in range(S // 512):
            xt_ = xTt.next()
            for j in range(4):
                tt = tb * 4 + j
                a, bb = xin.next(), xbf.next()
                ctx.dma('sp', 'x_in', a[:], xb_rows[tt * 128:(tt + 1) * 128, :], writes=[a], disjoint=False)
                if tt % 2 == 0:
                    ctx.op('dve', lambda g: g.tensor_copy(out=bb[:], in_=a[:]), reads=[a], writes=[bb])
                else:
                    ctx.op('act', lambda g: g.copy(out=bb[:], in_=a[:]), reads=[a], writes=[bb])
                for k4 in range(KC // 4):
                    ps = pst.next()
                    for kk in range(4):
                        kc = k4 * 4 + kk
                        ctx.op('pe', lambda g: g.transpose(out=ps[:, kk * 128:(kk + 1) * 128],
                                                           in_=bb[:, kc * 128:(kc + 1) * 128], identity=identb[:]),
                               reads=[bb, identb], writes=[ps], disjoint=(kk > 0))
                    eng = 'dve' if k4 % 2 == 0 else 'act'
                    src = ps[:, 0:512].rearrange("p (k t) -> p k t", k=4)
                    dst = xt_[:, k4 * 4:(k4 + 1) * 4, j * 128:(j + 1) * 128]
                    if eng == 'dve':
                        ctx.op('dve', lambda g: g.tensor_copy(out=dst, in_=src), reads=[ps], writes=[xt_],
                               disjoint=not (j == 0 and k4 == 0))
                    else:
                        ctx.op('act', lambda g: g.copy(out=dst, in_=src), reads=[ps], writes=[xt_], disjoint=True)
            ctx.dma('act', 'xT_out', xT_v[:, :, tb * 512:(tb + 1) * 512], xt_[:], reads=[xt_], writes=[r_xT])
        P.close()

        P = Pool_(ctx)
        xTb_r = P.ring(2, [128, KC, 512], BF16, 'xTb')
        wblk_r = P.ring(2, [128, KC, 512], BF16, 'wblk')
        psA = P.ring(4, [128, 512], F32, 'psA', psum=True)
        psF = P.ring(2, [128, 512], F32, 'psF', psum=True)
        oA = P.ring(3, [128, 512], BF16, 'oA')
        bv = P.sb([128, 2 * W], F32, 'bv')
        bf_ = P.sb([128, H], F32, 'bf')
        wf = P.sb([128, KC, 64], BF16, 'wf')
        carry = P.ring(2, [128, H], F32, 'carry')
        lf_r = P.ring(2, [128, H], F32, 'lf')
        lhi_r, llo_r = P.ring(2, [128, H], BF16, 'lhi'), P.ring(2, [128, H], BF16, 'llo')
        ctmp = P.ring(2, [128, H], F32, 'ctmp')
        cT_r = P.ring(2, [H, 128], F32, 'cT')
        ctx.dma('sp', 'const', bv[:, 0:W], b_in[0:1, 2 * W:3 * W].partition_broadcast(128), writes=[bv])
        ctx.dma('sp', 'const', bv[:, W:2 * W], b_in[0:1, 5 * W:6 * W].partition_broadcast(128), writes=[bv])
        ctx.dma('sp', 'const', bf_[:], b_in[0:1, 6 * W:6 * W + H].partition_broadcast(128), writes=[bf_])
        ctx.dma('sp', 'const', wf[:], wq_v[:, :, 6 * W:6 * W + 64], reads=[r_w], writes=[wf])
        bfT, nbfT = P.sb([H, 1], F32, 'bfT'), P.sb([H, 1], F32, 'nbfT')
        ctx.dma('sp', 'const', bfT[:], b_in[0, 6 * W:6 * W + H].rearrange("(h o) -> h o", o=1), writes=[bfT], allow_slow_non_contiguous=True)
        ctx.op('dve', lambda g: g.tensor_single_scalar(out=nbfT[:], in_=bfT[:], scalar=-1.0, op=ALU.mult), reads=[bfT], writes=[nbfT])
        onesH = P.sb([H, 512], F32, 'onesH')
        ctx.op('dve', lambda g: g.memset(onesH[:], 1.0), writes=[onesH])
        ef_r, cTt_r = P.ring(2, [H, 512], F32, 'ef'), P.ring(2, [H, 512], F32, 'cTt')
        cprev = None
        groupsA = [(0, qT, 0, r_q), (W, kT, 0, r_k), (3 * W, qT, W, r_q), (4 * W, kT, W, r_k)]
        groupsB = [(2 * W, 0), (5 * W, W)]
        for tb in range(S // 512):
            xTb = xTb_r.next()
            ctx.dma('sp', 'xTb_in', xTb[:], xT_v[:, :, tb * 512:(tb + 1) * 512], reads=[r_xT], writes=[xTb], disjoint=False)
            for (col0, dstT, drow, rres) in groupsA:
                for cb in range(W // 512):
                    wb = wblk_r.next()
                    cc0 = col0 + cb * 512
                    ctx.dma('sp', 'w_in', wb[:], wq_v[:, :, cc0:cc0 + 512], reads=[r_w], writes=[wb], disjoint=False)
                    for j in range(4):
                        ps = psA.next()
                        for kc in range(KC):
                            ctx.op('pe', lambda g: g.matmul(ps[:], lhsT=wb[:, kc, j * 128:(j + 1) * 128], rhs=xTb[:, kc, :],
                                                            start=(kc == 0), stop=(kc == KC - 1)),
                                   reads=[wb, xTb], writes=[ps], disjoint=(kc > 0))
                        o = oA.next()
                        bi = (cc0 + j * 128) // 128
                        ctx.op('act', lambda g: g.activation(out=o[:], in_=ps[:], func=AF.Identity,
                                                             bias=binT[:, bi:bi + 1], scale=1.0),
                               reads=[ps, binT], writes=[o])
                        r0 = drow + cb * 512 + j * 128
                        ctx.dma('act', 'qk_out', dstT[r0:r0 + 128, tb * 512:(tb + 1) * 512], o[:], reads=[o], writes=[rres])
            for (col0, dcol) in groupsB:
                for cb in range(W // 512):
                    wb = wblk_r.next()
                    cc0 = col0 + cb * 512
                    ctx.dma('sp', 'w_in', wb[:], wq_v[:, :, cc0:cc0 + 512], reads=[r_w], writes=[wb], disjoint=False)
                    for j in range(4):
                        ps = psA.next()
                        for kc in range(KC):
                            ctx.op('pe', lambda g: g.matmul(ps[:], lhsT=xTb[:, kc, j * 128:(j + 1) * 128], rhs=wb[:, kc, :],
                                                            start=(kc == 0), stop=(kc == KC - 1)),
                                   reads=[wb, xTb], writes=[ps], disjoint=(kc > 0))
                        o = oA.next()
                        bcol = dcol + cb * 512
                        ctx.op('dve', lambda g: g.tensor_tensor(out=o[:], in0=ps[:], in1=bv[:, bcol:bcol + 512], op=ALU.add),
                               reads=[ps, bv], writes=[o])
                        t0 = tb * 512 + j * 128
                        ctx.dma('act', 'v_out', vv[t0:t0 + 128, bcol:bcol + 512], o[:], reads=[o], writes=[r_v])
            psf = psA.next()
            for kc in range(KC):
                ctx.op('pe', lambda g: g.matmul(psf[:H, :], lhsT=wf[:, kc, 0:H], rhs=xTb[:, kc, :], start=(kc == 0), stop=(kc == KC - 1)),
                       reads=[wf, xTb], writes=[psf], disjoint=(kc > 0))
            ef = ef_r.next()
            ctx.op('act', lambda g: g.activation(out=ef[:], in_=psf[:H, :], func=AF.Exp, bias=nbfT[:, 0:1], scale=-1.0), reads=[psf, nbfT], writes=[ef])
            ctx.op('act', lambda g: g.activation(out=ef[:], in_=ef[:], func=AF.Ln, bias=1.0, scale=1.0), reads=[ef], writes=[ef])
            cTt = cTt_r.next()
            if tb == 0:
                ctx.op('dve', lambda g: g.tensor_tensor_scan(out=cTt[:], data0=onesH[:], data1=ef[:], initial=0.0, op0=ALU.mult, op1=ALU.subtract),
                       reads=[onesH, ef], writes=[cTt])
            else:
                ctx.op('dve', lambda g: g.tensor_tensor_scan(out=cTt[:], data0=onesH[:], data1=ef[:], initial=cprev[:, 511:512], op0=ALU.mult, op1=ALU.subtract),
                       reads=[onesH, ef, cprev], writes=[cTt])
            cprev = cTt
            ctx.dma('act', 'c_out', cfxT[:, tb * 512:(tb + 1) * 512], cTt[:], reads=[cTt], writes=[r_c])
        P.close()

        P = Pool_(ctx)
        mfx, msb = P.sb([128, 2048], F32, 'mfx'), P.sb([128, 2048], F32, 'msb')
        ones = P.sb([128, 512], F32, 'ones')
        ctx.dma('sp', 'const', mfx[:], c_mfx[:, :], writes=[mfx])
        ctx.dma('sp', 'const', msb[:], c_msb[:, :], writes=[msb])
        ctx.op('dve', lambda g: g.memset(ones[:], 1.0), writes=[ones])
        NQ = S // 128
        cq = P.sb([128, NQ, H], F32, 'cq')
        for t_ in range(NQ):
            ctx.dma('sp', 'att_in', cq[:, t_, :], cfxT[:, t_ * 128:(t_ + 1) * 128].rearrange("h p -> p h"), reads=[r_c], writes=[cq],
                    disjoint=(t_ > 0), allow_slow_non_contiguous=True)
        qh_r, kh_r = P.ring(2, [128, S], BF16, 'qh'), P.ring(2, [128, S], BF16, 'kh')
        vh_r = P.ring(2, [128, NQ, 128], BF16, 'vh')
        ck_r = P.ring(2, [128, S], F32, 'ck')
        oTh_r = P.ring(2, [128, S], BF16, 'oTh')
        ps_s = P.ring(4, [128, 512], F32, 'ps_s', psum=True)
        ps_t = P.ring(2, [128, 1024], BF16, 'ps_t', psum=True)
        ps_o = P.ring(2, [128, 512], F32, 'ps_o', psum=True)
        t1_r, t2_r, t3_r = P.ring(5, [128, 512], F32, 't1'), P.ring(4, [128, 512], F32, 't2'), P.ring(2, [128, 512], F32, 't3')
        p_r = P.ring(5, [128, 512], BF16, 'p')
        pT_r = P.ring(3, [128, 512], BF16, 'pT')
        rs_r = P.ring(6, [128, 16], F32, 'rs')
        sm_r = P.ring(16, [128, 2], F32, 'sm')
        on_r = P.ring(3, [128, 128], BF16, 'on')
        for br in range(2):
            for h in range(H):
                row0 = br * W + h * 128
                qh, kh, vh, oTh = qh_r.next(), kh_r.next(), vh_r.next(), oTh_r.next()
                ctx.dma('sp', 'att_in', qh[:], qT[row0:row0 + 128, :], reads=[r_q], writes=[qh], disjoint=False)
                ctx.dma('sp', 'att_in', kh[:], kT[row0:row0 + 128, :], reads=[r_k], writes=[kh], disjoint=False)
                ctx.dma('sp', 'att_in', vh[:], vv[:, row0:row0 + 128].rearrange("(t p) d -> p t d", p=128),
                        reads=[r_v], writes=[vh], disjoint=False)
                if br == 1:
                    ck = ck_r.next()
                    ctx.dma('sp', 'att_in', ck[:], cfxT[h:h + 1, :].partition_broadcast(128), reads=[r_c], writes=[ck], disjoint=False)
                blocks = []
                for i in range(NQ):
                    dblk = (i * 128) // 512
                    kbs = list(range(dblk + 1)) if br == 1 else list(range(dblk, -1, -1))
                    for n_, kb in enumerate(kbs):
                        blocks.append(dict(i=i, kb=kb, dblk=dblk, first=(n_ == 0), last=(n_ == len(kbs) - 1)))
                state = {}

                def stage_a1(bk):
                    i, kb, dblk = bk['i'], bk['kb'], bk['dblk']
                    im = i % 4
                    diag = (kb == dblk)
                    if bk['first']:
                        state[i] = dict(pso=ps_o.next(), rs=rs_r.next(), car=None)
                    pss = ps_s.next()
                    bk['pss'] = pss
                    ctx.op('pe', lambda g: g.matmul(pss[:], lhsT=qh[:, i * 128:(i + 1) * 128], rhs=kh[:, kb * 512:(kb + 1) * 512],
                                                    start=True, stop=True), reads=[qh, kh], writes=[pss])
                    t1 = t1_r.next()
                    bk['t1'] = t1
                    if br == 1:
                        ctx.op('dve', lambda g: g.scalar_tensor_tensor(out=t1[:], in0=pss[:], scalar=scale, in1=ck[:, kb * 512:(kb + 1) * 512],
                                                                       op0=ALU.mult, op1=ALU.subtract), reads=[pss, ck], writes=[t1])
                        if diag:
                            ctx.op('pool', lambda g: g.tensor_tensor(out=t1[:], in0=t1[:], in1=mfx[:, im * 512:(im + 1) * 512], op=ALU.add),
                                   reads=[t1, mfx], writes=[t1])
                    else:
                        ctx.op('act', lambda g: g.activation(out=t1[:], in_=pss[:], func=AF.Exp, scale=-scale), reads=[pss], writes=[t1])
                        ctx.op('act', lambda g: g.activation(out=t1[:], in_=t1[:], func=AF.Ln, bias=1.0, scale=1.0), reads=[t1], writes=[t1])

                def stage_a2(bk):
                    if br == 1:
                        return
                    i, kb, dblk = bk['i'], bk['kb'], bk['dblk']
                    im = i % 4
                    diag = (kb == dblk)
                    st = state[i]
                    pss, t1 = bk['pss'], bk['t1']
                    t2, t3 = t2_r.next(), t3_r.next()
                    bk['t2'] = t2
                    ctx.op('dve', lambda g: g.scalar_tensor_tensor(out=t2[:], in0=pss[:], scalar=-scale, in1=t1[:],
                                                                   op0=ALU.mult, op1=ALU.subtract), reads=[pss, t1], writes=[t2])
                    if diag:
                        ctx.op('pool', lambda g: g.tensor_tensor(out=t2[:], in0=t2[:], in1=msb[:, im * 512:(im + 1) * 512], op=ALU.mult),
                               reads=[t2, msb], writes=[t2])
                    ctx.op('dve', lambda g: g.tensor_tensor_scan(out=t3[:], data0=ones[:], data1=t2[:], initial=0.0,
                                                                 op0=ALU.mult, op1=ALU.add), reads=[ones, t2], writes=[t3])
                    sm = sm_r.next()
                    car = st['car']
                    if car is None:
                        ctx.op('dve', lambda g: g.tensor_copy(out=sm[:, 0:1], in_=t3[:, 511:512]), reads=[t3], writes=[sm])
                    else:
                        ctx.op('dve', lambda g: g.tensor_tensor(out=sm[:, 0:1], in0=t3[:, 511:512], in1=car[:, 0:1], op=ALU.add),
                               reads=[t3, car], writes=[sm])
                    st['car'] = sm
                    bk['sm'] = sm
                    ctx.op('pool', lambda g: g.tensor_tensor(out=t2[:], in0=t3[:], in1=t1[:], op=ALU.add), reads=[t3, t1], writes=[t2])

                def stage_a3(bk):
                    i, kb, dblk = bk['i'], bk['kb'], bk['dblk']
                    im = i % 4
                    diag = (kb == dblk)
                    st = state[i]
                    rs = st['rs']
                    p = p_r.next()
                    bk['p'] = p
                    if br == 1:
                        t1 = bk['t1']
                        ctx.op('act', lambda g: g.activation(out=p[:], in_=t1[:], func=AF.Exp, bias=cq[:, i, h:h + 1], scale=1.0,
                                                             accum_out=rs[:, kb:kb + 1]), reads=[t1, cq], writes=[p, rs], disjoint=True)
                    else:
                        t2, sm = bk['t2'], bk['sm']
                        if diag:
                            ctx.op('act', lambda g: g.activation(out=t2[:], in_=t2[:], func=AF.Exp, bias=sm[:, 0:1], scale=-1.0),
                                   reads=[t2, sm], writes=[t2])
                            ctx.op('pool', lambda g: g.tensor_tensor(out=p[:], in0=t2[:], in1=msb[:, im * 512:(im + 1) * 512], op=ALU.mult),
                                   reads=[t2, msb], writes=[p])
                        else:
                            ctx.op('act', lambda g: g.activation(out=p[:], in_=t2[:], func=AF.Exp, bias=sm[:, 0:1], scale=-1.0),
                                   reads=[t2, sm], writes=[p])

                def stage_b(bk):
                    i, kb, dblk = bk['i'], bk['kb'], bk['dblk']
                    im = i % 4
                    diag = (kb == dblk)
                    nj = (im + 1) if diag else 4
                    st = state[i]
                    pso, rs, p = st['pso'], st['rs'], bk['p']
                    pst_ = ps_t.next()
                    for j in range(nj):
                        ctx.op('pe', lambda g: g.transpose(out=pst_[:, j * 128:(j + 1) * 128], in_=p[:, j * 128:(j + 1) * 128], identity=identb[:]),
                               reads=[p, identb], writes=[pst_], disjoint=(j > 0))
                    pT = pT_r.next()
                    if br == 1:
                        ctx.op('dve', lambda g: g.tensor_copy(out=pT[:, :nj * 128], in_=pst_[:, :nj * 128]), reads=[pst_], writes=[pT])
                    else:
                        ctx.op('act', lambda g: g.copy(out=pT[:, :nj * 128], in_=pst_[:, :nj * 128]), reads=[pst_], writes=[pT])
                    for j in range(nj):
                        fm = bk['first'] and j == 0
                        lm = bk['last'] and (j == nj - 1)
                        ctx.op('pe', lambda g: g.matmul(pso[:, 0:128], lhsT=pT[:, j * 128:(j + 1) * 128], rhs=vh[:, kb * 4 + j, :],
                                                        start=fm, stop=lm), reads=[pT, vh], writes=[pso], disjoint=(not fm))
                    if not bk['last']:
                        return
                    on = on_r.next()
                    if br == 1:
                        sm = sm_r.next()
                        ctx.op('dve', lambda g: g.reduce_sum(out=sm[:, 0:1], in_=rs[:, 0:dblk + 1], axis=AX.X), reads=[rs], writes=[sm])
                        ctx.op('dve', lambda g: g.reciprocal(out=sm[:, 1:2], in_=sm[:, 0:1]), reads=[sm], writes=[sm])
                        ctx.op('dve', lambda g: g.tensor_single_scalar(out=on[:], in_=pso[:, 0:128], scalar=sm[:, 1:2], op=ALU.mult),
                               reads=[pso, sm], writes=[on])
                    else:
                        ctx.op('dve', lambda g: g.tensor_copy(out=on[:], in_=pso[:, 0:128]), reads=[pso], writes=[on])
                    pst2 = ps_t.next()
                    ctx.op('pe', lambda g: g.transpose(out=pst2[:, 0:128], in_=on[:], identity=identb[:]), reads=[on, identb], writes=[pst2])
                    ctx.op('act', lambda g: g.copy(out=oTh[:, i * 128:(i + 1) * 128], in_=pst2[:, 0:128]), reads=[pst2], writes=[oTh],
                           disjoint=(i > 0))
                    del state[i]

                stages = (stage_a1, stage_a2, stage_a3, stage_b)
                NB_ = len(blocks)
                for n_ in range(NB_ + len(stages) - 1):
                    for si, fn_ in enumerate(stages):
                        m_ = n_ - si
                        if 0 <= m_ < NB_:
                            fn_(blocks[m_])
                ctx.dma('act', 'oT_out', oT[row0:row0 + 128, :], oTh[:], reads=[oTh], writes=[r_oT])
        P.close()

        P = Pool_(ctx)
        TB = 512
        xTb_r = P.ring(1, [128, KC, TB], BF16, 'xTb')
        oTs_r, oTf_r = P.ring(1, [128, W // 128, TB], BF16, 'oTs'), P.ring(1, [128, W // 128, TB], BF16, 'oTf')
        wg1_r, wg2_r = P.ring(2, [128, KC, 256], BF16, 'wg1'), P.ring(2, [128, KC, 256], BF16, 'wg2')
        wb1_r, wb2_r = P.ring(2, [128, W // 128, 256], BF16, 'wb1'), P.ring(2, [128, W // 128, 256], BF16, 'wb2')
        psM = P.ring(8, [128, TB], F32, 'psM', psum=True)
        g1_r, g2_r = P.ring(2, [128, TB], F32, 'g1'), P.ring(2, [128, TB], F32, 'g2')
        mo_r = P.ring(3, [128, TB], BF16, 'mo')
        oT_v = oT.rearrange("(kc p) s -> p kc s", p=128)
        wbs_v, wbf_v = wbs.rearrange("(kc p) n -> p kc n", p=128), wbf.rearrange("(kc p) n -> p kc n", p=128)
        KW = W // 128
        for tb in range(S // TB):
            xTb, oTs, oTf = xTb_r.next(), oTs_r.next(), oTf_r.next()
            sl = slice(tb * TB, (tb + 1) * TB)
            ctx.dma('sp', 'm_in', xTb[:], xT_v[:, :, sl], reads=[r_xT], writes=[xTb], disjoint=False)
            ctx.dma('sp', 'm_in', oTs[:], oT_v[:, 0:KW, sl], reads=[r_oT], writes=[oTs], disjoint=False)
            ctx.dma('sp', 'm_in', oTf[:], oT_v[:, KW:2 * KW, sl], reads=[r_oT], writes=[oTf], disjoint=False)
            for c2 in range(D // 256):
                wg1, wg2, wb1, wb2 = wg1_r.next(), wg2_r.next(), wb1_r.next(), wb2_r.next()
                ctx.dma('sp', 'm_w', wg1[:], wq_v[:, :, c.G0 + c2 * 256:c.G0 + (c2 + 1) * 256], reads=[r_w], writes=[wg1], disjoint=False)
                ctx.dma('sp', 'm_w', wg2[:], wq_v[:, :, c.G0 + D + c2 * 256:c.G0 + D + (c2 + 1) * 256], reads=[r_w], writes=[wg2], disjoint=False)
                ctx.dma('sp', 'm_w', wb1[:], wbs_v[:, :, c2 * 256:(c2 + 1) * 256], reads=[r_w], writes=[wb1], disjoint=False)
                ctx.dma('sp', 'm_w', wb2[:], wbf_v[:, :, c2 * 256:(c2 + 1) * 256], reads=[r_w], writes=[wb2], disjoint=False)
                for jj in range(2):
                    ct_ = c2 * 2 + jj
                    cs = slice(jj * 128, (jj + 1) * 128)
                    pg1, pg2, pb1, pb2 = psM.next(), psM.next(), psM.next(), psM.next()
                    for (ps, wt, act_, nk) in ((pg1, wg1, xTb, KC), (pg2, wg2, xTb, KC), (pb1, wb1, oTs, KW), (pb2, wb2, oTf, KW)):
                        for kc in range(nk):
                            ctx.op('pe', lambda g: g.matmul(ps[:], lhsT=wt[:, kc, cs], rhs=act_[:, kc, :], start=(kc == 0), stop=(kc == nk - 1)),
                                   reads=[wt, act_], writes=[ps], disjoint=(kc > 0))
                    g1, g2 = g1_r.next(), g2_r.next()
                    ctx.op('act', lambda g: g.activation(out=g1[:], in_=pg1[:], func=AF.Sigmoid, bias=bgT[:, ct_:ct_ + 1], scale=1.0),
                           reads=[pg1, bgT], writes=[g1])
                    ctx.op('act', lambda g: g.activation(out=g2[:], in_=pg2[:], func=AF.Sigmoid, bias=bgT[:, D // 128 + ct_:D // 128 + ct_ + 1], scale=1.0),
                           reads=[pg2, bgT], writes=[g2])
                    ctx.op('dve', lambda g: g.tensor_tensor(out=g1[:], in0=g1[:], in1=pb1[:], op=ALU.mult), reads=[g1, pb1], writes=[g1])
                    ctx.op('dve', lambda g: g.tensor_tensor(out=g2[:], in0=g2[:], in1=pb2[:], op=ALU.mult), reads=[g2, pb2], writes=[g2])
                    mo = mo_r.next()
                    ctx.op('pool', lambda g: g.tensor_tensor(out=mo[:], in0=g1[:], in1=g2[:], op=ALU.add), reads=[g1, g2], writes=[mo])
                    ctx.dma('act', 'mT_out', mT[ct_ * 128:(ct_ + 1) * 128, sl], mo[:], reads=[mo], writes=[r_mT])
        P.close()

        P = Pool_(ctx)
        mTb_r = P.ring(2, [128, KC, 512], BF16, 'mTb')
        wo_r = P.ring(2, [128, KC, 512], BF16, 'wo')
        xr_r = P.ring(4, [128, 512], F32, 'xr')
        z_r = P.ring(4, [128, 512], F32, 'z')
        psZ = P.ring(4, [128, 512], F32, 'psZ', psum=True)
        mT_v = mT.rearrange("(kc p) s -> p kc s", p=128)
        wo_v = wo.rearrange("(kc p) n -> p kc n", p=128)
        for tb in range(S // 512):
            mTb = mTb_r.next()
            ctx.dma('sp', 'o_in', mTb[:], mT_v[:, :, tb * 512:(tb + 1) * 512], reads=[r_mT], writes=[mTb], disjoint=False)
            for cb in range(D // 512):
                wob = wo_r.next()
                ctx.dma('sp', 'o_w', wob[:], wo_v[:, :, cb * 512:(cb + 1) * 512], reads=[r_w], writes=[wob], disjoint=False)
                for j in range(4):
                    ps = psZ.next()
                    for kc in range(KC):
                        ctx.op('pe', lambda g: g.matmul(ps[:], lhsT=mTb[:, kc, j * 128:(j + 1) * 128], rhs=wob[:, kc, :],
                                                        start=(kc == 0), stop=(kc == KC - 1)), reads=[mTb, wob], writes=[ps], disjoint=(kc > 0))
                    t0 = tb * 512 + j * 128
                    xr, z = xr_r.next(), z_r.next()
                    ctx.dma('sp', 'o_x', xr[:], xb_rows[t0:t0 + 128, cb * 512:(cb + 1) * 512], writes=[xr], disjoint=False)
                    ctx.op('dve', lambda g: g.scalar_tensor_tensor(out=z[:], in0=xr[:], scalar=c.alpha, in1=ps[:], op0=ALU.mult, op1=ALU.add),
                           reads=[xr, ps], writes=[z])
                    ctx.dma('act', 'z_out', h1[b][t0:t0 + 128, cb * 512:(cb + 1) * 512], z[:], reads=[z], writes=[r_h1])
        P.close()

    def layer_norm(P, zt, gB, bB, st_r, mv_r):
        st, mv = st_r.next(), mv_r.next()
        nchunk = D // 512 if D >= 512 else 1
        cwid = D // nchunk
        for k in range(nchunk):
            ctx.op('dve', lambda g: g.bn_stats(out=st[:, k * 6:(k + 1) * 6], in_=zt[:, k * cwid:(k + 1) * cwid]), reads=[zt], writes=[st], disjoint=(k > 0))
        ctx.op('dve', lambda g: g.bn_aggr(out=mv[:, 0:2], in_=st[:, 0:nchunk * 6]), reads=[st], writes=[mv])
        ctx.op('dve', lambda g: g.tensor_single_scalar(out=mv[:, 3:4], in_=mv[:, 1:2], scalar=c.eps, op=ALU.add), reads=[mv], writes=[mv])
        ctx.op('act', lambda g: g.activation(out=mv[:, 3:4], in_=mv[:, 3:4], func=AF.Sqrt), reads=[mv], writes=[mv])
        ctx.op('dve', lambda g: g.reciprocal(out=mv[:, 2:3], in_=mv[:, 3:4]), reads=[mv], writes=[mv])
        ctx.op('dve', lambda g: g.tensor_scalar(out=zt[:], in0=zt[:], scalar1=mv[:, 0:1], scalar2=mv[:, 2:3], op0=ALU.subtract, op1=ALU.mult),
               reads=[zt, mv], writes=[zt])
        ctx.op('pool', lambda g: g.tensor_tensor(out=zt[:], in0=zt[:], in1=gB[:], op=ALU.mult), reads=[zt, gB], writes=[zt])
        ctx.op('pool', lambda g: g.tensor_tensor(out=zt[:], in0=zt[:], in1=bB[:], op=ALU.add), reads=[zt, bB], writes=[zt])

    NTT = T // 128
    PL = Pool_(ctx)
    lg_all = PL.sb([128, NTT, E], F32, 'lg_all')
    P = Pool_(ctx)
    g1B, b1B = P.sb([128, D], F32, 'g1B'), P.sb([128, D], F32, 'b1B')
    ctx.dma('sp', 'const', g1B[:], ln1_g[0:1, :].partition_broadcast(128), writes=[g1B])
    ctx.dma('sp', 'const', b1B[:], ln1_b[0:1, :].partition_broadcast(128), writes=[b1B])
    wr = P.sb([128, KC, E], F32, 'wr')
    ctx.dma('sp', 'const', wr[:], w_router.rearrange("(kc p) e -> p kc e", p=128), writes=[wr])
    brB = P.sb([128, E], F32, 'brB')
    ctx.dma('sp', 'const', brB[:], b_router[0:1, :].partition_broadcast(128), writes=[brB])
    zt_r = P.ring(2, [128, D], F32, 'zt')
    hbt_r = P.ring(2, [128, D], BF16, 'hbt')
    hT_r = P.ring(2, [128, KC, 128], F32, 'hT')
    st_r, mv_r = P.ring(2, [128, 48], F32, 'st'), P.ring(2, [128, 4], F32, 'mv')
    psT = P.ring(4, [128, 512], F32, 'psT', psum=True)
    psL = P.ring(2, [128, 512], F32, 'psL', psum=True)
    for b in range(BL):
        for tt in range(S // 128):
            zt = zt_r.next()
            rows = slice(tt * 128, (tt + 1) * 128)
            ctx.dma('sp', 'ln_in', zt[:], h1[b][rows, :], reads=[r_h1], writes=[zt], disjoint=False)
            layer_norm(P, zt, g1B, b1B, st_r, mv_r)
            ctx.dma('act', 'h_out', h1[b][rows, :], zt[:], reads=[zt], writes=[r_h1])
            hbt = hbt_r.next()
            ctx.op('act', lambda g: g.copy(out=hbt[:], in_=zt[:]), reads=[zt], writes=[hbt])
            ctx.dma('act', 'h_out', hb[b][rows, :], hbt[:], reads=[hbt], writes=[r_hb])
            hT = hT_r.next()
            for k4 in range(KC // 4):
                ps = psT.next()
                for kk in range(4):
                    kc = k4 * 4 + kk
                    ctx.op('pe', lambda g: g.transpose(out=ps[:, kk * 128:(kk + 1) * 128], in_=zt[:, kc * 128:(kc + 1) * 128], identity=identf[:]),
                           reads=[zt, identf], writes=[ps], disjoint=(kk > 0))
                ctx.op('dve', lambda g: g.tensor_copy(out=hT[:, k4 * 4:(k4 + 1) * 4, :], in_=ps[:, :].rearrange("p (k t) -> p k t", k=4)),
                       reads=[ps], writes=[hT], disjoint=(k4 > 0))
            psl = psL.next()
            for kc in range(KC):
                ctx.op('pe', lambda g: g.matmul(psl[:, 0:E], lhsT=hT[:, kc, :], rhs=wr[:, kc, :], start=(kc == 0), stop=(kc == KC - 1)),
                       reads=[hT, wr], writes=[psl], disjoint=(kc > 0))
            gi = b * (S // 128) + tt
            ctx.op('dve', lambda g: g.tensor_tensor(out=lg_all[:, gi, :], in0=psl[:, 0:E], in1=brB[:], op=ALU.add), reads=[psl, brB], writes=[lg_all], disjoint=True)
    P.close()

    NT = CH // 128
    NS = CAP // 128
    KF = DFF // 128
    nchunks = T // CH
    xe_l = [dscr("s_xe%d" % i, [E * CAP, D], BF16) for i in range(nchunks)]
    ye_l = [dscr("s_ye%d" % i, [E * CAP, D], F32) for i in range(nchunks)]
    r_xe_l, r_ye_l = [R("xe") for _ in range(nchunks)], [R("ye") for _ in range(nchunks)]
    bupT = PL.sb([128, E, 2 * KF], F32, 'bupT')
    ctx.dma('sp', 'const', bupT[:], b_up.rearrange("e (t p) -> p e t", p=128), writes=[bupT], allow_slow_non_contiguous=True)
    gat_l = [PL.sb([128, NT, 4], F32, 'gat') for _ in range(nchunks)]
    sloti_l = [PL.sb([128, NT * 4], I32, 'sloti') for _ in range(nchunks)]
    for ch in range(nchunks):
        b = (ch * CH) // S
        roff = (ch * CH) % S
        gat, sloti, xe_c, r_xe_c = gat_l[ch], sloti_l[ch], xe_l[ch], r_xe_l[ch]
        P = Pool_(ctx)
        ecap = P.sb([128, E], F32, 'ecap')
        ctx.dma('sp', 'const', ecap[:], c_ecap[:, :], writes=[ecap])
        top8 = P.sb([128, NT, 8], F32, 'top8')
        Mall = P.sb([128, NT, E], BF16, 'Mall')
        Mf = P.sb([128, NT, E], F32, 'Mf')
        OH = P.sb([128, 4, NT, E], F32, 'OH')
        posf = P.sb([128, NT, E], F32, 'posf')
        basef = P.sb([128, NT, E], F32, 'basef')
        slotf = P.sb([128, NT, 4], F32, 'slotf')
        tmpE = P.sb([128, NT, E], F32, 'tmpE')
        negm = P.sb([128, NT], F32, 'negm')
        den = P.sb([128, NT], F32, 'den')
        psR = P.ring(2, [128, NT * E], F32, 'psR', psum=True)
        for tt in range(NT):
            gi = ch * NT + tt
            lg = lg_all[:, gi, :]
            ctx.op('dve', lambda g: g.max(out=top8[:, tt, :], in_=lg), reads=[lg_all], writes=[top8], disjoint=(tt > 0))
            ctx.op('dve', lambda g: g.tensor_single_scalar(out=Mf[:, tt, :], in_=lg, scalar=top8[:, tt, 3:4], op=ALU.is_ge),
                   reads=[lg_all, top8], writes=[Mf], disjoint=(tt > 0))
            for k in range(4):
                ctx.op('dve', lambda g: g.tensor_single_scalar(out=OH[:, k, tt, :], in_=lg, scalar=top8[:, tt, k:k + 1], op=ALU.is_equal),
                       reads=[lg_all, top8], writes=[OH], disjoint=(tt > 0 or k > 0))
            ctx.op('dve', lambda g: g.tensor_single_scalar(out=negm[:, tt:tt + 1], in_=top8[:, tt, 0:1], scalar=-1.0, op=ALU.mult),
                   reads=[top8], writes=[negm], disjoint=(tt > 0))
            ctx.op('act', lambda g: g.activation(out=gat[:, tt, :], in_=top8[:, tt, 0:4], func=AF.Exp, bias=negm[:, tt:tt + 1], scale=1.0,
                                                 accum_out=den[:, tt:tt + 1]), reads=[top8, negm], writes=[gat, den], disjoint=(tt > 0))
            ctx.op('dve', lambda g: g.reciprocal(out=den[:, tt:tt + 1], in_=den[:, tt:tt + 1]), reads=[den], writes=[den], disjoint=True)
            ctx.op('dve', lambda g: g.tensor_single_scalar(out=gat[:, tt, :], in_=gat[:, tt, :], scalar=den[:, tt:tt + 1], op=ALU.mult),
                   reads=[gat, den], writes=[gat], disjoint=True)
        ctx.op('dve', lambda g: g.tensor_copy(out=Mall[:], in_=Mf[:]), reads=[Mf], writes=[Mall])
        Mflat = Mall[:, :, :].rearrange("p t e -> p (t e)")
        ps1, ps2 = psR.next(), psR.next()
        ctx.op('pe', lambda g: g.matmul(ps1[:], lhsT=trib[:, 128:256], rhs=Mflat, start=True, stop=True), reads=[trib, Mall], writes=[ps1])
        ctx.op('pe', lambda g: g.matmul(ps2[:], lhsT=trib[:, 256:384], rhs=Mflat, start=True, stop=True), reads=[trib, Mall], writes=[ps2])
        ps2v = ps2[:, :].rearrange("p (t e) -> p t e", e=E)
        ctx.op('dve', lambda g: g.memset(basef[:, 0, :], 0.0), writes=[basef])
        for tt in range(1, NT):
            ctx.op('dve', lambda g: g.tensor_tensor(out=basef[:, tt, :], in0=basef[:, tt - 1, :], in1=ps2v[:, tt - 1, :], op=ALU.add),
                   reads=[basef, ps2], writes=[basef], disjoint=True)
        ctx.op('dve', lambda g: g.tensor_tensor(out=posf[:], in0=basef[:], in1=ps1[:, :].rearrange("p (t e) -> p t e", e=E), op=ALU.add),
               reads=[basef, ps1], writes=[posf])
        ctx.op('dve', lambda g: g.tensor_single_scalar(out=posf[:], in_=posf[:], scalar=float(CAP - 1), op=ALU.min), reads=[posf], writes=[posf])
        for tt in range(NT):
            ctx.op('dve', lambda g: g.tensor_tensor(out=posf[:, tt, :], in0=posf[:, tt, :], in1=ecap[:], op=ALU.add), reads=[posf, ecap], writes=[posf], disjoint=True)
        for k in range(4):
            ctx.op('dve', lambda g: g.tensor_tensor(out=tmpE[:], in0=OH[:, k, :, :], in1=posf[:], op=ALU.mult), reads=[OH, posf], writes=[tmpE])
            ctx.op('dve', lambda g: g.reduce_sum(out=slotf[:, :, k], in_=tmpE[:], axis=AX.X), reads=[tmpE], writes=[slotf], disjoint=(k > 0))
        ctx.op('dve', lambda g: g.tensor_copy(out=sloti[:], in_=slotf[:, :, :].rearrange("p t k -> p (t k)")), reads=[slotf], writes=[sloti])
        hbt_r = P.ring(3, [128, D], BF16, 'hbt')
        for tt in range(NT):
            hbt = hbt_r.next()
            rows = slice(roff + tt * 128, roff + (tt + 1) * 128)
            ctx.dma('sp', 'd_in', hbt[:], hb[b][rows, :], reads=[r_hb], writes=[hbt], disjoint=False)
            for k in range(4):
                col = tt * 4 + k
                ctx.dma('pool', 'scat', None, None, reads=[hbt, sloti], writes=[r_xe_c],
                        fn=lambda g: g.indirect_dma_start(out=xe_c[:, :], out_offset=bass.IndirectOffsetOnAxis(ap=sloti[:, col:col + 1], axis=0),
                                                          in_=hbt[:], in_offset=None))
        P.close()
    P = Pool_(ctx)
    xs_r = P.ring(1, [128, D], BF16, 'xs')
    xeT_l = [P.sb([128, KC, CAP], BF16, 'xeT') for _ in range(nchunks)]
    aT_l = [P.sb([128, KF, CAP], BF16, 'aT') for _ in range(nchunks)]
    stg_r = P.ring(3, [128, 4096], F32, 'stg')
    wb_r = P.ring(4, [128, 4096], BF16, 'wb')
    bd_r = P.ring(2, [128, 256], F32, 'bd')
    y_r = P.ring(3, [128, 256], F32, 'y')
    e1_r, e2_r, e3_r = P.ring(2, [128, CAP], F32, 'e1'), P.ring(2, [128, CAP], F32, 'e2'), P.ring(2, [128, CAP], F32, 'e3')
    psX = P.ring(2, [128, 1024], BF16, 'psX', psum=True)
    psU = P.ring(4, [128, 512], F32, 'psU', psum=True)
    psD = P.ring(2, [128, 512], F32, 'psD', psum=True)
    assert KC * 128 == 4096 or KC * 128 <= 4096
    ccnt = [0]

    def load_cast(src_ap, nk, ncol):
        st_, wb_ = stg_r.next(), wb_r.next()
        sv = st_[:, 0:nk * ncol].rearrange("p (k n) -> p k n", n=ncol)
        wv = wb_[:, 0:nk * ncol].rearrange("p (k n) -> p k n", n=ncol)
        ctx.dma('sp', 'e_w', sv, src_ap, writes=[st_], disjoint=False)
        eng = ('act', 'dve', 'act')[ccnt[0] % 3]
        ccnt[0] += 1
        if eng == 'act':
            ctx.op('act', lambda g: g.copy(out=wb_[:, 0:nk * ncol], in_=st_[:, 0:nk * ncol]), reads=[st_], writes=[wb_])
        else:
            ctx.op('dve', lambda g: g.tensor_copy(out=wb_[:, 0:nk * ncol], in_=st_[:, 0:nk * ncol]), reads=[st_], writes=[wb_])
        return wb_, wv

    for e_ in range(E):
        wu_e = w_up[e_ * D:(e_ + 1) * D, :].rearrange("(kc p) n -> p kc n", p=128)
        wd_e = w_down[e_ * DFF:(e_ + 1) * DFF, :].rearrange("(kc p) n -> p kc n", p=128)
        for ch in range(nchunks):
            xT_e = xeT_l[ch]
            for st_i in range(NS):
                xs = xs_r.next()
                r0 = e_ * CAP + st_i * 128
                ctx.dma('sp', 'e_in', xs[:], xe_l[ch][r0:r0 + 128, :], reads=[r_xe_l[ch]], writes=[xs], disjoint=False)
                for k4 in range(KC // 4):
                    ps = psX.next()
                    for kk in range(4):
                        kc = k4 * 4 + kk
                        ctx.op('pe', lambda g: g.transpose(out=ps[:, kk * 128:(kk + 1) * 128], in_=xs[:, kc * 128:(kc + 1) * 128], identity=identb[:]),
                               reads=[xs, identb], writes=[ps], disjoint=(kk > 0))
                    dst = xT_e[:, k4 * 4:(k4 + 1) * 4, st_i * 128:(st_i + 1) * 128]
                    src = ps[:, 0:512].rearrange("p (k t) -> p k t", k=4)
                    ctx.op('dve', lambda g: g.tensor_copy(out=dst, in_=src), reads=[ps], writes=[xT_e], disjoint=not (st_i == 0 and k4 == 0))
        for j in range(KF):
            wgR, wg = load_cast(wu_e[:, :, j * 128:(j + 1) * 128], KC, 128)
            wuR, wu = load_cast(wu_e[:, :, DFF + j * 128:DFF + (j + 1) * 128], KC, 128)
            for ch in range(nchunks):
                xT_e, aT = xeT_l[ch], aT_l[ch]
                pg, pu = psU.next(), psU.next()
                for (ps, wR, wt) in ((pg, wgR, wg), (pu, wuR, wu)):
                    for kc in range(KC):
                        ctx.op('pe', lambda g: g.matmul(ps[:, :CAP], lhsT=wt[:, kc, :], rhs=xT_e[:, kc, :], start=(kc == 0), stop=(kc == KC - 1)),
                               reads=[wR, xT_e], writes=[ps], disjoint=(kc > 0))
                e1, e2, e3 = e1_r.next(), e2_r.next(), e3_r.next()
                ctx.op('dve', lambda g: g.tensor_scalar(out=e1[:], in0=pg[:, :CAP], scalar1=bupT[:, e_, j:j + 1], scalar2=7.0, op0=ALU.add, op1=ALU.min),
                       reads=[pg, bupT], writes=[e1])
                ctx.op('act', lambda g: g.activation(out=e2[:], in_=e1[:], func=AF.Sigmoid, scale=1.702), reads=[e1], writes=[e2])
                ctx.op('dve', lambda g: g.tensor_scalar(out=e3[:], in0=pu[:, :CAP], scalar1=bupT[:, e_, KF + j:KF + j + 1], scalar2=7.0, op0=ALU.add, op1=ALU.min),
                       reads=[pu, bupT], writes=[e3])
                ctx.op('pool', lambda g: g.tensor_scalar(out=e3[:], in0=e3[:], scalar1=-7.0, scalar2=1.0, op0=ALU.max, op1=ALU.add), reads=[e3], writes=[e3])
                ctx.op('pool', lambda g: g.tensor_tensor(out=e1[:], in0=e1[:], in1=e2[:], op=ALU.mult), reads=[e1, e2], writes=[e1])
                ctx.op('dve', lambda g: g.tensor_tensor(out=aT[:, j, :], in0=e1[:], in1=e3[:], op=ALU.mult), reads=[e1, e3], writes=[aT], disjoint=(j > 0))
        for cb in range(D // 256):
            wdR, wd = load_cast(wd_e[:, :, cb * 256:(cb + 1) * 256], KF, 256)
            bd = bd_r.next()
            ctx.dma('sp', 'e_w', bd[:], b_down[e_:e_ + 1, cb * 256:(cb + 1) * 256].partition_broadcast(128), writes=[bd], disjoint=False)
            for ch in range(nchunks):
                aT = aT_l[ch]
                for st_i in range(NS):
                    ps = psD.next()
                    for kc in range(KF):
                        ctx.op('pe', lambda g: g.matmul(ps[:, 0:256], lhsT=aT[:, kc, st_i * 128:(st_i + 1) * 128], rhs=wd[:, kc, :], start=(kc == 0), stop=(kc == KF - 1)),
                               reads=[aT, wdR], writes=[ps], disjoint=(kc > 0))
                    y = y_r.next()
                    ctx.op('dve', lambda g: g.tensor_tensor(out=y[:], in0=ps[:, 0:256], in1=bd[:], op=ALU.add), reads=[ps, bd], writes=[y])
                    r0 = e_ * CAP + st_i * 128
                    ctx.dma('act', 'y_out', ye_l[ch][r0:r0 + 128, cb * 256:(cb + 1) * 256], y[:], reads=[y], writes=[r_ye_l[ch]])
    P.close()
    P = Pool_(ctx)
    g2B, b2B = P.sb([128, D], F32, 'g2B'), P.sb([128, D], F32, 'b2B')
    ctx.dma('sp', 'const', g2B[:], ln2_g[0:1, :].partition_broadcast(128), writes=[g2B])
    ctx.dma('sp', 'const', b2B[:], ln2_b[0:1, :].partition_broadcast(128), writes=[b2B])
    yg_r = P.ring(3, [128, D], F32, 'yg')
    acc_r = P.ring(2, [128, D], F32, 'acc')
    hr_r = P.ring(2, [128, D], F32, 'hr')
    st_r, mv_r = P.ring(2, [128, 48], F32, 'st'), P.ring(2, [128, 4], F32, 'mv')
    for ch in range(nchunks):
        b = (ch * CH) // S
        roff = (ch * CH) % S
        gat, sloti, ye_c, r_ye_c = gat_l[ch], sloti_l[ch], ye_l[ch], r_ye_l[ch]
        for tt in range(NT):
            acc = acc_r.next()
            for k in range(4):
                yg = yg_r.next()
                col = tt * 4 + k
                ctx.dma('pool', 'gath', None, None, reads=[r_ye_c, sloti], writes=[yg], disjoint=False,
                        fn=lambda g: g.indirect_dma_start(out=yg[:], out_offset=None, in_=ye_c[:, :],
                                                          in_offset=bass.IndirectOffsetOnAxis(ap=sloti[:, col:col + 1], axis=0)))
                if k == 0:
                    ctx.op('dve', lambda g: g.tensor_single_scalar(out=acc[:], in_=yg[:], scalar=gat[:, tt, 0:1], op=ALU.mult),
                           reads=[yg, gat], writes=[acc])
                else:
                    ctx.op('dve', lambda g: g.scalar_tensor_tensor(out=acc[:], in0=yg[:], scalar=gat[:, tt, k:k + 1], in1=acc[:], op0=ALU.mult, op1=ALU.add),
                           reads=[yg, gat, acc], writes=[acc])
            hr = hr_r.next()
            rows = slice(roff + tt * 128, roff + (tt + 1) * 128)
            ctx.dma('sp', 'c_in', hr[:], h1[b][rows, :], reads=[r_h1], writes=[hr], disjoint=False)
            ctx.op('dve', lambda g: g.scalar_tensor_tensor(out=acc[:], in0=hr[:], scalar=c.alpha, in1=acc[:], op0=ALU.mult, op1=ALU.add),
                   reads=[hr, acc], writes=[acc])
            layer_norm(P, acc, g2B, b2B, st_r, mv_r)
            orow = ch * CH + tt * 128
            ctx.dma('act', 'out', out[orow:orow + 128, :], acc[:], reads=[acc], writes=[r_out])
    P.close()
    PL.close()
    P0.close()
    ctx.barrier()
    es.close()
    return nc, ctx


def host_consts(cfg):
    E, CAP = cfg.E, cfg.CAP
    cs = {}
    cs["c_identb"] = np.eye(128, dtype=np.float32).astype(ml_dtypes.bfloat16)
    cs["c_identf"] = np.eye(128, dtype=np.float32)
    q = np.arange(128)[:, None]
    k = np.arange(512)[None, :]
    mf = np.zeros((128, 4 * 512), np.float32)
    ms = np.zeros((128, 4 * 512), np.float32)
    for im in range(4):
        qpos = im * 128 + q
        mf[:, im * 512:(im + 1) * 512] = np.where(k <= qpos, 0.0, -30000.0)
        ms[:, im * 512:(im + 1) * 512] = np.where(k < qpos, 1.0, 0.0)
    cs["c_mfx"], cs["c_msb"] = mf, ms
    j = np.arange(128)[:, None]
    i = np.arange(128)[None, :]
    cs["c_tri"] = np.concatenate([(j <= i), (j < i), np.ones((128, 128), bool)], axis=1).astype(np.float32)
    cs["c_ecap"] = np.tile((np.arange(E) * CAP).astype(np.float32)[None, :], (128, 1))
    return cs


def run(cfg, ncores, inputs, trace=False, debug=False):
    nc, ctx = build(cfg, ncores, debug)
    c = cfg
    T, D, E, DFF = c.T, c.D, c.E, c.DFF
    cs = host_consts(cfg)
    f = lambda a: np.ascontiguousarray(np.asarray(a, dtype=np.float32))
    xs = f(inputs["x"]).reshape(ncores, T, D)
    shared = {
        "w_in": f(inputs["w_in"])[0], "b_in": f(inputs["b_in"]).reshape(1, -1),
        "w_branch_sb": f(inputs["w_branch_sb"])[0], "w_branch_fx": f(inputs["w_branch_fx"])[0],
        "w_out": f(inputs["w_out"])[0], "ln1_g": f(inputs["ln1_g"]).reshape(1, -1), "ln1_b": f(inputs["ln1_b"]).reshape(1, -1),
        "w_router": f(inputs["w_router"])[0], "b_router": f(inputs["b_router"]).reshape(1, -1),
        "w_up": f(inputs["w_up"]).reshape(E * D, 2 * DFF), "b_up": f(inputs["b_up"]).reshape(E, 2 * DFF),
        "w_down": f(inputs["w_down"]).reshape(E * DFF, D), "b_down": f(inputs["b_down"]).reshape(E, D),
        "ln2_g": f(inputs["ln2_g"]).reshape(1, -1), "ln2_b": f(inputs["ln2_b"]).reshape(1, -1),
    }
    shared.update(cs)
    in_maps = [dict(shared, x=xs[i]) for i in range(ncores)]
    res = run_bass_kernel_spmd(nc, in_maps, core_ids=list(range(ncores)), **({"trace": True} if trace else {}))
    outs = [np.asarray(r["out"], dtype=np.float32) for r in res.results]
    return np.concatenate(outs, axis=0), res


def kernel(**inputs):
    B, S, D = inputs["x"].shape
    cfg = Cfg(D=D, S=S, BL=B // NCORES)
    o, _ = run(cfg, NCORES, inputs)
    return o.reshape(B, S, D)
```

```python
import numpy as np
import ml_dtypes
from contextlib import ExitStack
import concourse.bass as bass
import concourse.mybir as mybir
from concourse.bass_utils import run_bass_kernel_spmd

F32, BF16, I32 = mybir.dt.float32, mybir.dt.bfloat16, mybir.dt.int32
AF = mybir.ActivationFunctionType
ALU = mybir.AluOpType
AX = mybir.AxisListType

NCORES = 4
DEBUG_NAMES = ('s_dbg', 's_dbg2', 's_dbg3', 's_xT', 's_qT', 's_kT', 's_v', 's_cfx', 's_cfxT', 's_oT', 's_mT', 's_h1_0', 's_hb_0', 's_xe', 's_ye', 's_wq')


class Cfg:
    def __init__(self, D=4096, S=4096, BL=4, H=16, E=32, CH=2048, CAP=384):
        self.D, self.S, self.BL, self.H, self.E, self.CH, self.CAP = D, S, BL, H, E, CH, CAP
        self.W = H * 128
        self.DFF = D // 2
        self.PROJ = 6 * self.W + H + 2 * D
        self.KC = D // 128
        self.T = BL * S
        self.G0 = 6 * self.W + H
        self.alpha = 2.0 ** 0.25
        self.eps = 1e-5


class Res:
    def __init__(self, name, obj=None):
        self.name, self.obj, self.w, self.r = name, obj, {}, {}
        self.dsem = None

    def __getitem__(self, idx):
        return self.obj[idx]


class Ctx:
    def __init__(self, nc, es):
        self.nc, self.es = nc, es
        self.eng = {'pe': nc.tensor, 'act': nc.scalar, 'dve': nc.vector, 'pool': nc.gpsimd, 'sp': nc.sync}
        self.psem = {k: es.enter_context(nc.semaphore('p_' + k)) for k in self.eng}
        self.pcnt = {k: 0 for k in self.eng}
        self.waited = {k: {} for k in self.eng}
        self.dsem = {}
        self.sem_all, self.sem_free = [], []
        self.nins = 0

    def borrow_sem(self):
        if self.sem_free:
            return self.sem_free.pop()
        key = 'd_%d' % len(self.sem_all)
        ent = [self.es.enter_context(self.nc.semaphore(key)), 0, key]
        self.sem_all.append(ent)
        return ent

    def _wait(self, e, events, own_ok=True):
        for name, (sem, val) in events.items():
            if own_ok and name == 'p_' + e and e in ('pe', 'sp'):
                continue
            if self.waited[e].get(name, 0) >= val:
                continue
            self.eng[e].wait_ge(sem, val)
            self.waited[e][name] = val

    def _deps(self, e, reads, writes, disjoint, own_ok=True):
        for r in reads:
            self._wait(e, r.w, own_ok)
        for w in writes:
            self._wait(e, w.r, own_ok)
            self._wait(e, w.w, own_ok)

    def _mark(self, ev, reads, writes, disjoint):
        for r in reads:
            r.r[ev[0]] = ev[1]
        for w in writes:
            if not disjoint:
                w.w = {}
                w.r = {}
            w.w[ev[0]] = ev[1]

    def op(self, e, fn, reads=(), writes=(), disjoint=False):
        self._deps(e, reads, writes, disjoint)
        ins = fn(self.eng[e])
        self.pcnt[e] += 1
        ins.then_inc(self.psem[e], 1)
        self._mark(('p_' + e, (self.psem[e], self.pcnt[e])), reads, writes, disjoint)
        self.nins += 1
        return ins

    def dma(self, q, stream, out, in_, reads=(), writes=(), disjoint=True, fn=None, **kw):
        side = [r_ for r_ in list(writes) + list(reads) if r_.obj is not None]
        assert side, stream
        owner = side[0]
        if owner.dsem is None:
            owner.dsem = self.borrow_sem()
        ent = owner.dsem
        key = ent[2]
        self._deps(q, reads, writes, disjoint, own_ok=False)
        if fn is None:
            ins = self.eng[q].dma_start(out=out, in_=in_, **kw)
        else:
            ins = fn(self.eng[q])
        ent[1] += 16
        ins.then_inc(ent[0], 16)
        self._mark((key, (ent[0], ent[1])), reads, writes, disjoint)
        self.nins += 1
        return ins

    def barrier(self):
        evs = {'p_' + k: (self.psem[k], self.pcnt[k]) for k in self.eng if self.pcnt[k] > 0}
        for ent in self.sem_all:
            if ent[1] > 0:
                evs[ent[2]] = (ent[0], ent[1])
        for e in self.eng:
            self._wait(e, evs)


class Pool_:
    def __init__(self, ctx):
        self.ctx, self.es = ctx, ExitStack()
        self.n = 0
        self.res = []

    def sb(self, shape, dt, name='t'):
        self.n += 1
        t = self.es.enter_context(self.ctx.nc.sbuf_tensor('%s_%d_%d' % (name, id(self) % 9973, self.n), list(shape), dt))
        r_ = Res(name, t)
        self.res.append(r_)
        return r_

    def ps(self, shape, dt, name='ps'):
        self.n += 1
        t = self.es.enter_context(self.ctx.nc.psum_tensor('%s_%d_%d' % (name, id(self) % 9973, self.n), list(shape), dt))
        return Res(name, t)

    def ring(self, n, shape, dt, name='r', psum=False):
        return Ring([(self.ps if psum else self.sb)(shape, dt, name) for _ in range(n)])

    def close(self):
        self.ctx.barrier()
        for r_ in self.res:
            if r_.dsem is not None:
                self.ctx.sem_free.append(r_.dsem)
                r_.dsem = None
        self.es.close()


class Ring:
    def __init__(self, items):
        self.items, self.i = items, -1

    def next(self):
        self.i = (self.i + 1) % len(self.items)
        return self.items[self.i]


def build(cfg, ncores, debug=False):
    c = cfg
    D, S, BL, H, E, W, DFF, KC, T, CAP, CH = c.D, c.S, c.BL, c.H, c.E, c.W, c.DFF, c.KC, c.T, c.CAP, c.CH
    nc = bass.Bass("TRN2", target_bir_lowering=False)
    dt_in = lambda n, s, d=F32: nc.dram_tensor(n, list(s), d, kind="ExternalInput").ap()
    x = dt_in("x", [T, D])
    w_in = dt_in("w_in", [D, c.PROJ])
    b_in = dt_in("b_in", [1, c.PROJ])
    w_bs = dt_in("w_branch_sb", [W, D])
    w_bf = dt_in("w_branch_fx", [W, D])
    w_out = dt_in("w_out", [D, D])
    ln1_g, ln1_b = dt_in("ln1_g", [1, D]), dt_in("ln1_b", [1, D])
    w_router, b_router = dt_in("w_router", [D, E]), dt_in("b_router", [1, E])
    w_up, b_up = dt_in("w_up", [E * D, 2 * DFF]), dt_in("b_up", [E, 2 * DFF])
    w_down, b_down = dt_in("w_down", [E * DFF, D]), dt_in("b_down", [E, D])
    ln2_g, ln2_b = dt_in("ln2_g", [1, D]), dt_in("ln2_b", [1, D])
    c_identb = dt_in("c_identb", [128, 128], BF16)
    c_identf = dt_in("c_identf", [128, 128])
    c_mfx = dt_in("c_mfx", [128, 4 * 512])
    c_msb = dt_in("c_msb", [128, 4 * 512])
    c_tri = dt_in("c_tri", [128, 3 * 128])
    c_ecap = dt_in("c_ecap", [128, E])
    out = nc.dram_tensor("out", [T, D], F32, kind="ExternalOutput").ap()

    dscr = lambda n, s, d: (nc.dram_tensor(n, list(s), d, kind='ExternalOutput').ap() if (debug and n in DEBUG_NAMES) else nc.dram_tensor(n, list(s), d).ap())
    wq = dscr("s_wq", [D, c.PROJ], BF16)
    wbs, wbf, wo = dscr("s_wbs", [W, D], BF16), dscr("s_wbf", [W, D], BF16), dscr("s_wo", [D, D], BF16)
    xT = dscr("s_xT", [D, S], BF16)
    qT, kT = dscr("s_qT", [2 * W, S], BF16), dscr("s_kT", [2 * W, S], BF16)
    vv = dscr("s_v", [S, 2 * W], BF16)
    cfx, cfxT = dscr("s_cfx", [S, H], F32), dscr("s_cfxT", [H, S], F32)
    oT = dscr("s_oT", [2 * W, S], BF16)
    dbg = dscr("s_dbg", [S, 3 * H], F32)
    dbg2 = dscr("s_dbg2", [128, 5 * 512], F32)
    dbg3 = dscr("s_dbg3", [128, 3 * 512], BF16)
    mT = dscr("s_mT", [D, S], BF16)
    h1 = [dscr("s_h1_%d" % b, [S, D], F32) for b in range(BL)]
    hb = [dscr("s_hb_%d" % b, [S, D], BF16) for b in range(BL)]

    es = ExitStack()
    ctx = Ctx(nc, es)
    R = lambda n: Res(n)
    r_w = R("weights")
    r_xT, r_q, r_k, r_v, r_c, r_oT, r_mT = R("xT"), R("qT"), R("kT"), R("v"), R("c"), R("oT"), R("mT")
    r_h1, r_hb, r_xe, r_ye, r_out = R("h1"), R("hb"), R("xe"), R("ye"), R("out")
    scale = 128 ** -0.5

    P0 = Pool_(ctx)
    identb, identf = P0.sb([128, 128], BF16, 'identb'), P0.sb([128, 128], F32, 'identf')
    tri = P0.sb([128, 384], F32, 'tri')
    trib = P0.sb([128, 384], BF16, 'trib')
    ctx.dma('sp', 'const', identb[:], c_identb[:, :], writes=[identb])
    ctx.dma('sp', 'const', identf[:], c_identf[:, :], writes=[identf])
    ctx.dma('sp', 'const', tri[:], c_tri[:, :], writes=[tri])
    ctx.op('dve', lambda e: e.tensor_copy(out=trib[:], in_=tri[:]), reads=[tri], writes=[trib])

    def cast_matrix(P, src, dst, rows, cols, rings, cnt):
        src_v = src.rearrange("(r p) c -> r p c", p=128)
        dst_v = dst.rearrange("(r p) c -> r p c", p=128)
        cw = min(cols, 4096)
        for r in range(rows // 128):
            for c0 in range(0, cols, cw):
                cc = min(cw, cols - c0)
                a, b_ = rings[0].next(), rings[1].next()
                ctx.dma('sp', 'cast_in', a[:, :cc], src_v[r, :, c0:c0 + cc], writes=[a], disjoint=False)
                e = ('dve', 'act')[cnt[0] % 2]
                cnt[0] += 1
                if e == 'act':
                    ctx.op(e, lambda g: g.copy(out=b_[:, :cc], in_=a[:, :cc]), reads=[a], writes=[b_])
                else:
                    ctx.op(e, lambda g: g.tensor_copy(out=b_[:, :cc], in_=a[:, :cc]), reads=[a], writes=[b_])
                ctx.dma('act', 'cast_out', dst_v[r, :, c0:c0 + cc], b_[:, :cc], reads=[b_], writes=[r_w])

    P = Pool_(ctx)
    rings = (P.ring(3, [128, 4096], F32, 'ci'), P.ring(3, [128, 4096], BF16, 'co'))
    cnt = [0]
    cast_matrix(P, w_in, wq, D, c.PROJ, rings, cnt)
    cast_matrix(P, w_bs, wbs, W, D, rings, cnt)
    cast_matrix(P, w_bf, wbf, W, D, rings, cnt)
    cast_matrix(P, w_out, wo, D, D, rings, cnt)
    P.close()

    wq_v = wq.rearrange("(kc p) n -> p kc n", p=128)
    xT_v = xT.rearrange("(kc p) s -> p kc s", p=128)

    NB = (6 * W) // 128
    NG = (2 * D) // 128
    binT = P0.sb([128, NB], F32, 'binT')
    bgT = P0.sb([128, NG], F32, 'bgT')
    ctx.dma('sp', 'const', binT[:], b_in[0, 0:6 * W].rearrange("(t p) -> p t", p=128), writes=[binT],
            allow_slow_non_contiguous=True)
    ctx.dma('sp', 'const', bgT[:], b_in[0, c.G0:c.G0 + 2 * D].rearrange("(t p) -> p t", p=128), writes=[bgT],
            allow_slow_non_contiguous=True)

    for b in range(BL):
        xb_rows = x[b * S:(b + 1) * S, :]
        P = Pool_(ctx)
        xin = P.ring(2, [128, D], F32, 'xin')
        xbf = P.ring(2, [128, D], BF16, 'xbf')
        xTt = P.ring(2, [128, KC, 512], BF16, 'xTt')
        pst = P.ring(4, [128, 1024], BF16, 'pst', psum=True)
        for tb in range(S // 512):
            xt_ = xTt.next()
            for j in range(4):
                tt = tb * 4 + j
                a, bb = xin.next(), xbf.next()
                ctx.dma('sp', 'x_in', a[:], xb_rows[tt * 128:(tt + 1) * 128, :], writes=[a], disjoint=False)
                if tt % 2 == 0:
                    ctx.op('dve', lambda g: g.tensor_copy(out=bb[:], in_=a[:]), reads=[a], writes=[bb])
                else:
                    ctx.op('act', lambda g: g.copy(out=bb[:], in_=a[:]), reads=[a], writes=[bb])
                for k4 in range(KC // 4):
                    ps = pst.next()
                    for kk in range(4):
                        kc = k4 * 4 + kk
                        ctx.op('pe', lambda g: g.transpose(out=ps[:, kk * 128:(kk + 1) * 128],
                                                           in_=bb[:, kc * 128:(kc + 1) * 128], identity=identb[:]),
                               reads=[bb, identb], writes=[ps], disjoint=(kk > 0))
                    eng = 'dve' if k4 % 2 == 0 else 'act'
                    src = ps[:, 0:512].rearrange("p (k t) -> p k t", k=4)
                    dst = xt_[:, k4 * 4:(k4 + 1) * 4, j * 128:(j + 1) * 128]
                    if eng == 'dve':
                        ctx.op('dve', lambda g: g.tensor_copy(out=dst, in_=src), reads=[ps], writes=[xt_],
                               disjoint=not (j == 0 and k4 == 0))
                    else:
                        ctx.op('act', lambda g: g.copy(out=dst, in_=src), reads=[ps], writes=[xt_], disjoint=True)
            ctx.dma('act', 'xT_out', xT_v[:, :, tb * 512:(tb + 1) * 512], xt_[:], reads=[xt_], writes=[r_xT])
        P.close()

        P = Pool_(ctx)
        xTb_r = P.ring(2, [128, KC, 512], BF16, 'xTb')
        wblk_r = P.ring(2, [128, KC, 512], BF16, 'wblk')
        psA = P.ring(4, [128, 512], F32, 'psA', psum=True)
        psF = P.ring(2, [128, 512], F32, 'psF', psum=True)
        oA = P.ring(3, [128, 512], BF16, 'oA')
        bv = P.sb([128, 2 * W], F32, 'bv')
        bf_ = P.sb([128, H], F32, 'bf')
        wf = P.sb([128, KC, 64], BF16, 'wf')
        carry = P.ring(2, [128, H], F32, 'carry')
        lf_r = P.ring(2, [128, H], F32, 'lf')
        lhi_r, llo_r = P.ring(2, [128, H], BF16, 'lhi'), P.ring(2, [128, H], BF16, 'llo')
        ctmp = P.ring(2, [128, H], F32, 'ctmp')
        cT_r = P.ring(2, [H, 128], F32, 'cT')
        ctx.dma('sp', 'const', bv[:, 0:W], b_in[0:1, 2 * W:3 * W].partition_broadcast(128), writes=[bv])
        ctx.dma('sp', 'const', bv[:, W:2 * W], b_in[0:1, 5 * W:6 * W].partition_broadcast(128), writes=[bv])
        ctx.dma('sp', 'const', bf_[:], b_in[0:1, 6 * W:6 * W + H].partition_broadcast(128), writes=[bf_])
        ctx.dma('sp', 'const', wf[:], wq_v[:, :, 6 * W:6 * W + 64], reads=[r_w], writes=[wf])
        bfT, nbfT = P.sb([H, 1], F32, 'bfT'), P.sb([H, 1], F32, 'nbfT')
        ctx.dma('sp', 'const', bfT[:], b_in[0, 6 * W:6 * W + H].rearrange("(h o) -> h o", o=1), writes=[bfT], allow_slow_non_contiguous=True)
        ctx.op('dve', lambda g: g.tensor_single_scalar(out=nbfT[:], in_=bfT[:], scalar=-1.0, op=ALU.mult), reads=[bfT], writes=[nbfT])
        onesH = P.sb([H, 512], F32, 'onesH')
        ctx.op('dve', lambda g: g.memset(onesH[:], 1.0), writes=[onesH])
        ef_r, cTt_r = P.ring(2, [H, 512], F32, 'ef'), P.ring(2, [H, 512], F32, 'cTt')
        cprev = None
        groupsA = [(0, qT, 0, r_q), (W, kT, 0, r_k), (3 * W, qT, W, r_q), (4 * W, kT, W, r_k)]
        groupsB = [(2 * W, 0), (5 * W, W)]
        for tb in range(S // 512):
            xTb = xTb_r.next()
            ctx.dma('sp', 'xTb_in', xTb[:], xT_v[:, :, tb * 512:(tb + 1) * 512], reads=[r_xT], writes=[xTb], disjoint=False)
            for (col0, dstT, drow, rres) in groupsA:
                for cb in range(W // 512):
                    wb = wblk_r.next()
                    cc0 = col0 + cb * 512
                    ctx.dma('sp', 'w_in', wb[:], wq_v[:, :, cc0:cc0 + 512], reads=[r_w], writes=[wb], disjoint=False)
                    for j in range(4):
                        ps = psA.next()
                        for kc in range(KC):
                            ctx.op('pe', lambda g: g.matmul(ps[:], lhsT=wb[:, kc, j * 128:(j + 1) * 128], rhs=xTb[:, kc, :],
                                                            start=(kc == 0), stop=(kc == KC - 1)),
                                   reads=[wb, xTb], writes=[ps], disjoint=(kc > 0))
                        o = oA.next()
                        bi = (cc0 + j * 128) // 128
                        ctx.op('act', lambda g: g.activation(out=o[:], in_=ps[:], func=AF.Identity,
                                                             bias=binT[:, bi:bi + 1], scale=1.0),
                               reads=[ps, binT], writes=[o])
                        r0 = drow + cb * 512 + j * 128
                        ctx.dma('act', 'qk_out', dstT[r0:r0 + 128, tb * 512:(tb + 1) * 512], o[:], reads=[o], writes=[rres])
            for (col0, dcol) in groupsB:
                for cb in range(W // 512):
                    wb = wblk_r.next()
                    cc0 = col0 + cb * 512
                    ctx.dma('sp', 'w_in', wb[:], wq_v[:, :, cc0:cc0 + 512], reads=[r_w], writes=[wb], disjoint=False)
                    for j in range(4):
                        ps = psA.next()
                        for kc in range(KC):
                            ctx.op('pe', lambda g: g.matmul(ps[:], lhsT=xTb[:, kc, j * 128:(j + 1) * 128], rhs=wb[:, kc, :],
                                                            start=(kc == 0), stop=(kc == KC - 1)),
                                   reads=[wb, xTb], writes=[ps], disjoint=(kc > 0))
                        o = oA.next()
                        bcol = dcol + cb * 512
                        ctx.op('dve', lambda g: g.tensor_tensor(out=o[:], in0=ps[:], in1=bv[:, bcol:bcol + 512], op=ALU.add),
                               reads=[ps, bv], writes=[o])
                        t0 = tb * 512 + j * 128
                        ctx.dma('act', 'v_out', vv[t0:t0 + 128, bcol:bcol + 512], o[:], reads=[o], writes=[r_v])
            psf = psA.next()
            for kc in range(KC):
                ctx.op('pe', lambda g: g.matmul(psf[:H, :], lhsT=wf[:, kc, 0:H], rhs=xTb[:, kc, :], start=(kc == 0), stop=(kc == KC - 1)),
                       reads=[wf, xTb], writes=[psf], disjoint=(kc > 0))
            ef = ef_r.next()
            ctx.op('act', lambda g: g.activation(out=ef[:], in_=psf[:H, :], func=AF.Exp, bias=nbfT[:, 0:1], scale=-1.0), reads=[psf, nbfT], writes=[ef])
            ctx.op('act', lambda g: g.activation(out=ef[:], in_=ef[:], func=AF.Ln, bias=1.0, scale=1.0), reads=[ef], writes=[ef])
            cTt = cTt_r.next()
            if tb == 0:
                ctx.op('dve', lambda g: g.tensor_tensor_scan(out=cTt[:], data0=onesH[:], data1=ef[:], initial=0.0, op0=ALU.mult, op1=ALU.subtract),
                       reads=[onesH, ef], writes=[cTt])
            else:
                ctx.op('dve', lambda g: g.tensor_tensor_scan(out=cTt[:], data0=onesH[:], data1=ef[:], initial=cprev[:, 511:512], op0=ALU.mult, op1=ALU.subtract),
                       reads=[onesH, ef, cprev], writes=[cTt])
            cprev = cTt
            ctx.dma('act', 'c_out', cfxT[:, tb * 512:(tb + 1) * 512], cTt[:], reads=[cTt], writes=[r_c])
        P.close()

        P = Pool_(ctx)
        mfx, msb = P.sb([128, 2048], F32, 'mfx'), P.sb([128, 2048], F32, 'msb')
        ones = P.sb([128, 512], F32, 'ones')
        ctx.dma('sp', 'const', mfx[:], c_mfx[:, :], writes=[mfx])
        ctx.dma('sp', 'const', msb[:], c_msb[:, :], writes=[msb])
        ctx.op('dve', lambda g: g.memset(ones[:], 1.0), writes=[ones])
        NQ = S // 128
        cq = P.sb([128, NQ, H], F32, 'cq')
        for t_ in range(NQ):
            ctx.dma('sp', 'att_in', cq[:, t_, :], cfxT[:, t_ * 128:(t_ + 1) * 128].rearrange("h p -> p h"), reads=[r_c], writes=[cq],
                    disjoint=(t_ > 0), allow_slow_non_contiguous=True)
        qh_r, kh_r = P.ring(2, [128, S], BF16, 'qh'), P.ring(2, [128, S], BF16, 'kh')
        vh_r = P.ring(2, [128, NQ, 128], BF16, 'vh')
        ck_r = P.ring(2, [128, S], F32, 'ck')
        oTh_r = P.ring(2, [128, S], BF16, 'oTh')
        ps_s = P.ring(4, [128, 512], F32, 'ps_s', psum=True)
        ps_t = P.ring(2, [128, 1024], BF16, 'ps_t', psum=True)
        ps_o = P.ring(2, [128, 512], F32, 'ps_o', psum=True)
        t1_r, t2_r, t3_r = P.ring(5, [128, 512], F32, 't1'), P.ring(4, [128, 512], F32, 't2'), P.ring(2, [128, 512], F32, 't3')
        p_r = P.ring(5, [128, 512], BF16, 'p')
        pT_r = P.ring(3, [128, 512], BF16, 'pT')
        rs_r = P.ring(6, [128, 16], F32, 'rs')
        sm_r = P.ring(16, [128, 2], F32, 'sm')
        on_r = P.ring(3, [128, 128], BF16, 'on')
        for br in range(2):
            for h in range(H):
                row0 = br * W + h * 128
                qh, kh, vh, oTh = qh_r.next(), kh_r.next(), vh_r.next(), oTh_r.next()
                ctx.dma('sp', 'att_in', qh[:], qT[row0:row0 + 128, :], reads=[r_q], writes=[qh], disjoint=False)
                ctx.dma('sp', 'att_in', kh[:], kT[row0:row0 + 128, :], reads=[r_k], writes=[kh], disjoint=False)
                ctx.dma('sp', 'att_in', vh[:], vv[:, row0:row0 + 128].rearrange("(t p) d -> p t d", p=128),
                        reads=[r_v], writes=[vh], disjoint=False)
                if br == 1:
                    ck = ck_r.next()
                    ctx.dma('sp', 'att_in', ck[:], cfxT[h:h + 1, :].partition_broadcast(128), reads=[r_c], writes=[ck], disjoint=False)
                blocks = []
                for i in range(NQ):
                    dblk = (i * 128) // 512
                    kbs = list(range(dblk + 1)) if br == 1 else list(range(dblk, -1, -1))
                    for n_, kb in enumerate(kbs):
                        blocks.append(dict(i=i, kb=kb, dblk=dblk, first=(n_ == 0), last=(n_ == len(kbs) - 1)))
                state = {}

                def stage_a1(bk):
                    i, kb, dblk = bk['i'], bk['kb'], bk['dblk']
                    im = i % 4
                    diag = (kb == dblk)
                    if bk['first']:
                        state[i] = dict(pso=ps_o.next(), rs=rs_r.next(), car=None)
                    pss = ps_s.next()
                    bk['pss'] = pss
                    ctx.op('pe', lambda g: g.matmul(pss[:], lhsT=qh[:, i * 128:(i + 1) * 128], rhs=kh[:, kb * 512:(kb + 1) * 512],
                                                    start=True, stop=True), reads=[qh, kh], writes=[pss])
                    t1 = t1_r.next()
                    bk['t1'] = t1
                    if br == 1:
                        ctx.op('dve', lambda g: g.scalar_tensor_tensor(out=t1[:], in0=pss[:], scalar=scale, in1=ck[:, kb * 512:(kb + 1) * 512],
                                                                       op0=ALU.mult, op1=ALU.subtract), reads=[pss, ck], writes=[t1])
                        if diag:
                            ctx.op('pool', lambda g: g.tensor_tensor(out=t1[:], in0=t1[:], in1=mfx[:, im * 512:(im + 1) * 512], op=ALU.add),
                                   reads=[t1, mfx], writes=[t1])
                    else:
                        ctx.op('act', lambda g: g.activation(out=t1[:], in_=pss[:], func=AF.Exp, scale=-scale), reads=[pss], writes=[t1])
                        ctx.op('act', lambda g: g.activation(out=t1[:], in_=t1[:], func=AF.Ln, bias=1.0, scale=1.0), reads=[t1], writes=[t1])

                def stage_a2(bk):
                    if br == 1:
                        return
                    i, kb, dblk = bk['i'], bk['kb'], bk['dblk']
                    im = i % 4
                    diag = (kb == dblk)
                    st = state[i]
                    pss, t1 = bk['pss'], bk['t1']
                    t2, t3 = t2_r.next(), t3_r.next()
                    bk['t2'] = t2
                    ctx.op('dve', lambda g: g.scalar_tensor_tensor(out=t2[:], in0=pss[:], scalar=-scale, in1=t1[:],
                                                                   op0=ALU.mult, op1=ALU.subtract), reads=[pss, t1], writes=[t2])
                    if diag:
                        ctx.op('pool', lambda g: g.tensor_tensor(out=t2[:], in0=t2[:], in1=msb[:, im * 512:(im + 1) * 512], op=ALU.mult),
                               reads=[t2, msb], writes=[t2])
                    ctx.op('dve', lambda g: g.tensor_tensor_scan(out=t3[:], data0=ones[:], data1=t2[:], initial=0.0,
                                                                 op0=ALU.mult, op1=ALU.add), reads=[ones, t2], writes=[t3])
                    sm = sm_r.next()
                    car = st['car']
                    if car is None:
                        ctx.op('dve', lambda g: g.tensor_copy(out=sm[:, 0:1], in_=t3[:, 511:512]), reads=[t3], writes=[sm])
                    else:
                        ctx.op('dve', lambda g: g.tensor_tensor(out=sm[:, 0:1], in0=t3[:, 511:512], in1=car[:, 0:1], op=ALU.add),
                               reads=[t3, car], writes=[sm])
                    st['car'] = sm
                    bk['sm'] = sm
                    ctx.op('pool', lambda g: g.tensor_tensor(out=t2[:], in0=t3[:], in1=t1[:], op=ALU.add), reads=[t3, t1], writes=[t2])

                def stage_a3(bk):
                    i, kb, dblk = bk['i'], bk['kb'], bk['dblk']
                    im = i % 4
                    diag = (kb == dblk)
                    st = state[i]
                    rs = st['rs']
                    p = p_r.next()
                    bk['p'] = p
                    if br == 1:
                        t1 = bk['t1']
                        ctx.op('act', lambda g: g.activation(out=p[:], in_=t1[:], func=AF.Exp, bias=cq[:, i, h:h + 1], scale=1.0,
                                                             accum_out=rs[:, kb:kb + 1]), reads=[t1, cq], writes=[p, rs], disjoint=True)
                    else:
                        t2, sm = bk['t2'], bk['sm']
                        if diag:
                            ctx.op('act', lambda g: g.activation(out=t2[:], in_=t2[:], func=AF.Exp, bias=sm[:, 0:1], scale=-1.0),
                                   reads=[t2, sm], writes=[t2])
                            ctx.op('pool', lambda g: g.tensor_tensor(out=p[:], in0=t2[:], in1=msb[:, im * 512:(im + 1) * 512], op=ALU.mult),
                                   reads=[t2, msb], writes=[p])
                        else:
                            ctx.op('act', lambda g: g.activation(out=p[:], in_=t2[:], func=AF.Exp, bias=sm[:, 0:1], scale=-1.0),
                                   reads=[t2, sm], writes=[p])

                def stage_b(bk):
                    i, kb, dblk = bk['i'], bk['kb'], bk['dblk']
                    im = i % 4
                    diag = (kb == dblk)
                    nj = (im + 1) if diag else 4
                    st = state[i]
                    pso, rs, p = st['pso'], st['rs'], bk['p']
                    pst_ = ps_t.next()
                    for j in range(nj):
                        ctx.op('pe', lambda g: g.transpose(out=pst_[:, j * 128:(j + 1) * 128], in_=p[:, j * 128:(j + 1) * 128], identity=identb[:]),
                               reads=[p, identb], writes=[pst_], disjoint=(j > 0))
                    pT = pT_r.next()
                    if br == 1:
                        ctx.op('dve', lambda g: g.tensor_copy(out=pT[:, :nj * 128], in_=pst_[:, :nj * 128]), reads=[pst_], writes=[pT])
                    else:
                        ctx.op('act', lambda g: g.copy(out=pT[:, :nj * 128], in_=pst_[:, :nj * 128]), reads=[pst_], writes=[pT])
                    for j in range(nj):
                        fm = bk['first'] and j == 0
                        lm = bk['last'] and (j == nj - 1)
                        ctx.op('pe', lambda g: g.matmul(pso[:, 0:128], lhsT=pT[:, j * 128:(j + 1) * 128], rhs=vh[:, kb * 4 + j, :],
                                                        start=fm, stop=lm), reads=[pT, vh], writes=[pso], disjoint=(not fm))
                    if not bk['last']:
                        return
                    on = on_r.next()
                    if br == 1:
                        sm = sm_r.next()
                        ctx.op('dve', lambda g: g.reduce_sum(out=sm[:, 0:1], in_=rs[:, 0:dblk + 1], axis=AX.X), reads=[rs], writes=[sm])
                        ctx.op('dve', lambda g: g.reciprocal(out=sm[:, 1:2], in_=sm[:, 0:1]), reads=[sm], writes=[sm])
                        ctx.op('dve', lambda g: g.tensor_single_scalar(out=on[:], in_=pso[:, 0:128], scalar=sm[:, 1:2], op=ALU.mult),
                               reads=[pso, sm], writes=[on])
                    else:
                        ctx.op('dve', lambda g: g.tensor_copy(out=on[:], in_=pso[:, 0:128]), reads=[pso], writes=[on])
                    pst2 = ps_t.next()
                    ctx.op('pe', lambda g: g.transpose(out=pst2[:, 0:128], in_=on[:], identity=identb[:]), reads=[on, identb], writes=[pst2])
                    ctx.op('act', lambda g: g.copy(out=oTh[:, i * 128:(i + 1) * 128], in_=pst2[:, 0:128]), reads=[pst2], writes=[oTh],
                           disjoint=(i > 0))
                    del state[i]

                stages = (stage_a1, stage_a2, stage_a3, stage_b)
                NB_ = len(blocks)
                for n_ in range(NB_ + len(stages) - 1):
                    for si, fn_ in enumerate(stages):
                        m_ = n_ - si
                        if 0 <= m_ < NB_:
                            fn_(blocks[m_])
                ctx.dma('act', 'oT_out', oT[row0:row0 + 128, :], oTh[:], reads=[oTh], writes=[r_oT])
        P.close()

        P = Pool_(ctx)
        TB = 512
        xTb_r = P.ring(1, [128, KC, TB], BF16, 'xTb')
        oTs_r, oTf_r = P.ring(1, [128, W // 128, TB], BF16, 'oTs'), P.ring(1, [128, W // 128, TB], BF16, 'oTf')
        wg1_r, wg2_r = P.ring(2, [128, KC, 256], BF16, 'wg1'), P.ring(2, [128, KC, 256], BF16, 'wg2')
        wb1_r, wb2_r = P.ring(2, [128, W // 128, 256], BF16, 'wb1'), P.ring(2, [128, W // 128, 256], BF16, 'wb2')
        psM = P.ring(8, [128, TB], F32, 'psM', psum=True)
        g1_r, g2_r = P.ring(2, [128, TB], F32, 'g1'), P.ring(2, [128, TB], F32, 'g2')
        mo_r = P.ring(3, [128, TB], BF16, 'mo')
        oT_v = oT.rearrange("(kc p) s -> p kc s", p=128)
        wbs_v, wbf_v = wbs.rearrange("(kc p) n -> p kc n", p=128), wbf.rearrange("(kc p) n -> p kc n", p=128)
        KW = W // 128
        for tb in range(S // TB):
            xTb, oTs, oTf = xTb_r.next(), oTs_r.next(), oTf_r.next()
            sl = slice(tb * TB, (tb + 1) * TB)
            ctx.dma('sp', 'm_in', xTb[:], xT_v[:, :, sl], reads=[r_xT], writes=[xTb], disjoint=False)
            ctx.dma('sp', 'm_in', oTs[:], oT_v[:, 0:KW, sl], reads=[r_oT], writes=[oTs], disjoint=False)
            ctx.dma('sp', 'm_in', oTf[:], oT_v[:, KW:2 * KW, sl], reads=[r_oT], writes=[oTf], disjoint=False)
            for c2 in range(D // 256):
                wg1, wg2, wb1, wb2 = wg1_r.next(), wg2_r.next(), wb1_r.next(), wb2_r.next()
                ctx.dma('sp', 'm_w', wg1[:], wq_v[:, :, c.G0 + c2 * 256:c.G0 + (c2 + 1) * 256], reads=[r_w], writes=[wg1], disjoint=False)
                ctx.dma('sp', 'm_w', wg2[:], wq_v[:, :, c.G0 + D + c2 * 256:c.G0 + D + (c2 + 1) * 256], reads=[r_w], writes=[wg2], disjoint=False)
                ctx.dma('sp', 'm_w', wb1[:], wbs_v[:, :, c2 * 256:(c2 + 1) * 256], reads=[r_w], writes=[wb1], disjoint=False)
                ctx.dma('sp', 'm_w', wb2[:], wbf_v[:, :, c2 * 256:(c2 + 1) * 256], reads=[r_w], writes=[wb2], disjoint=False)
                for jj in range(2):
                    ct_ = c2 * 2 + jj
                    cs = slice(jj * 128, (jj + 1) * 128)
                    pg1, pg2, pb1, pb2 = psM.next(), psM.next(), psM.next(), psM.next()
                    for (ps, wt, act_, nk) in ((pg1, wg1, xTb, KC), (pg2, wg2, xTb, KC), (pb1, wb1, oTs, KW), (pb2, wb2, oTf, KW)):
                        for kc in range(nk):
                            ctx.op('pe', lambda g: g.matmul(ps[:], lhsT=wt[:, kc, cs], rhs=act_[:, kc, :], start=(kc == 0), stop=(kc == nk - 1)),
                                   reads=[wt, act_], writes=[ps], disjoint=(kc > 0))
                    g1, g2 = g1_r.next(), g2_r.next()
                    ctx.op('act', lambda g: g.activation(out=g1[:], in_=pg1[:], func=AF.Sigmoid, bias=bgT[:, ct_:ct_ + 1], scale=1.0),
                           reads=[pg1, bgT], writes=[g1])
                    ctx.op('act', lambda g: g.activation(out=g2[:], in_=pg2[:], func=AF.Sigmoid, bias=bgT[:, D // 128 + ct_:D // 128 + ct_ + 1], scale=1.0),
                           reads=[pg2, bgT], writes=[g2])
                    ctx.op('dve', lambda g: g.tensor_tensor(out=g1[:], in0=g1[:], in1=pb1[:], op=ALU.mult), reads=[g1, pb1], writes=[g1])
                    ctx.op('dve', lambda g: g.tensor_tensor(out=g2[:], in0=g2[:], in1=pb2[:], op=ALU.mult), reads=[g2, pb2], writes=[g2])
                    mo = mo_r.next()
                    ctx.op('pool', lambda g: g.tensor_tensor(out=mo[:], in0=g1[:], in1=g2[:], op=ALU.add), reads=[g1, g2], writes=[mo])
                    ctx.dma('act', 'mT_out', mT[ct_ * 128:(ct_ + 1) * 128, sl], mo[:], reads=[mo], writes=[r_mT])
        P.close()

        P = Pool_(ctx)
        mTb_r = P.ring(2, [128, KC, 512], BF16, 'mTb')
        wo_r = P.ring(2, [128, KC, 512], BF16, 'wo')
        xr_r = P.ring(4, [128, 512], F32, 'xr')
        z_r = P.ring(4, [128, 512], F32, 'z')
        psZ = P.ring(4, [128, 512], F32, 'psZ', psum=True)
        mT_v = mT.rearrange("(kc p) s -> p kc s", p=128)
        wo_v = wo.rearrange("(kc p) n -> p kc n", p=128)
        for tb in range(S // 512):
            mTb = mTb_r.next()
            ctx.dma('sp', 'o_in', mTb[:], mT_v[:, :, tb * 512:(tb + 1) * 512], reads=[r_mT], writes=[mTb], disjoint=False)
            for cb in range(D // 512):
                wob = wo_r.next()
                ctx.dma('sp', 'o_w', wob[:], wo_v[:, :, cb * 512:(cb + 1) * 512], reads=[r_w], writes=[wob], disjoint=False)
                for j in range(4):
                    ps = psZ.next()
                    for kc in range(KC):
                        ctx.op('pe', lambda g: g.matmul(ps[:], lhsT=mTb[:, kc, j * 128:(j + 1) * 128], rhs=wob[:, kc, :],
                                                        start=(kc == 0), stop=(kc == KC - 1)), reads=[mTb, wob], writes=[ps], disjoint=(kc > 0))
                    t0 = tb * 512 + j * 128
                    xr, z = xr_r.next(), z_r.next()
                    ctx.dma('sp', 'o_x', xr[:], xb_rows[t0:t0 + 128, cb * 512:(cb + 1) * 512], writes=[xr], disjoint=False)
                    ctx.op('dve', lambda g: g.scalar_tensor_tensor(out=z[:], in0=xr[:], scalar=c.alpha, in1=ps[:], op0=ALU.mult, op1=ALU.add),
                           reads=[xr, ps], writes=[z])
                    ctx.dma('act', 'z_out', h1[b][t0:t0 + 128, cb * 512:(cb + 1) * 512], z[:], reads=[z], writes=[r_h1])
        P.close()

    def layer_norm(P, zt, gB, bB, st_r, mv_r):
        st, mv = st_r.next(), mv_r.next()
        nchunk = D // 512 if D >= 512 else 1
        cwid = D // nchunk
        for k in range(nchunk):
            ctx.op('dve', lambda g: g.bn_stats(out=st[:, k * 6:(k + 1) * 6], in_=zt[:, k * cwid:(k + 1) * cwid]), reads=[zt], writes=[st], disjoint=(k > 0))
        ctx.op('dve', lambda g: g.bn_aggr(out=mv[:, 0:2], in_=st[:, 0:nchunk * 6]), reads=[st], writes=[mv])
        ctx.op('dve', lambda g: g.tensor_single_scalar(out=mv[:, 3:4], in_=mv[:, 1:2], scalar=c.eps, op=ALU.add), reads=[mv], writes=[mv])
        ctx.op('act', lambda g: g.activation(out=mv[:, 3:4], in_=mv[:, 3:4], func=AF.Sqrt), reads=[mv], writes=[mv])
        ctx.op('dve', lambda g: g.reciprocal(out=mv[:, 2:3], in_=mv[:, 3:4]), reads=[mv], writes=[mv])
        ctx.op('dve', lambda g: g.tensor_scalar(out=zt[:], in0=zt[:], scalar1=mv[:, 0:1], scalar2=mv[:, 2:3], op0=ALU.subtract, op1=ALU.mult),
               reads=[zt, mv], writes=[zt])
        ctx.op('pool', lambda g: g.tensor_tensor(out=zt[:], in0=zt[:], in1=gB[:], op=ALU.mult), reads=[zt, gB], writes=[zt])
        ctx.op('pool', lambda g: g.tensor_tensor(out=zt[:], in0=zt[:], in1=bB[:], op=ALU.add), reads=[zt, bB], writes=[zt])

    NTT = T // 128
    PL = Pool_(ctx)
    lg_all = PL.sb([128, NTT, E], F32, 'lg_all')
    P = Pool_(ctx)
    g1B, b1B = P.sb([128, D], F32, 'g1B'), P.sb([128, D], F32, 'b1B')
    ctx.dma('sp', 'const', g1B[:], ln1_g[0:1, :].partition_broadcast(128), writes=[g1B])
    ctx.dma('sp', 'const', b1B[:], ln1_b[0:1, :].partition_broadcast(128), writes=[b1B])
    wr = P.sb([128, KC, E], F32, 'wr')
    ctx.dma('sp', 'const', wr[:], w_router.rearrange("(kc p) e -> p kc e", p=128), writes=[wr])
    brB = P.sb([128, E], F32, 'brB')
    ctx.dma('sp', 'const', brB[:], b_router[0:1, :].partition_broadcast(128), writes=[brB])
    zt_r = P.ring(2, [128, D], F32, 'zt')
    hbt_r = P.ring(2, [128, D], BF16, 'hbt')
    hT_r = P.ring(2, [128, KC, 128], F32, 'hT')
    st_r, mv_r = P.ring(2, [128, 48], F32, 'st'), P.ring(2, [128, 4], F32, 'mv')
    psT = P.ring(4, [128, 512], F32, 'psT', psum=True)
    psL = P.ring(2, [128, 512], F32, 'psL', psum=True)
    for b in range(BL):
        for tt in range(S // 128):
            zt = zt_r.next()
            rows = slice(tt * 128, (tt + 1) * 128)
            ctx.dma('sp', 'ln_in', zt[:], h1[b][rows, :], reads=[r_h1], writes=[zt], disjoint=False)
            layer_norm(P, zt, g1B, b1B, st_r, mv_r)
            ctx.dma('act', 'h_out', h1[b][rows, :], zt[:], reads=[zt], writes=[r_h1])
            hbt = hbt_r.next()
            ctx.op('act', lambda g: g.copy(out=hbt[:], in_=zt[:]), reads=[zt], writes=[hbt])
            ctx.dma('act', 'h_out', hb[b][rows, :], hbt[:], reads=[hbt], writes=[r_hb])
            hT = hT_r.next()
            for k4 in range(KC // 4):
                ps = psT.next()
                for kk in range(4):
                    kc = k4 * 4 + kk
                    ctx.op('pe', lambda g: g.transpose(out=ps[:, kk * 128:(kk + 1) * 128], in_=zt[:, kc * 128:(kc + 1) * 128], identity=identf[:]),
                           reads=[zt, identf], writes=[ps], disjoint=(kk > 0))
                ctx.op('dve', lambda g: g.tensor_copy(out=hT[:, k4 * 4:(k4 + 1) * 4, :], in_=ps[:, :].rearrange("p (k t) -> p k t", k=4)),
                       reads=[ps], writes=[hT], disjoint=(k4 > 0))
            psl = psL.next()
            for kc in range(KC):
                ctx.op('pe', lambda g: g.matmul(psl[:, 0:E], lhsT=hT[:, kc, :], rhs=wr[:, kc, :], start=(kc == 0), stop=(kc == KC - 1)),
                       reads=[hT, wr], writes=[psl], disjoint=(kc > 0))
            gi = b * (S // 128) + tt
            ctx.op('dve', lambda g: g.tensor_tensor(out=lg_all[:, gi, :], in0=psl[:, 0:E], in1=brB[:], op=ALU.add), reads=[psl, brB], writes=[lg_all], disjoint=True)
    P.close()

    NT = CH // 128
    NS = CAP // 128
    KF = DFF // 128
    nchunks = T // CH
    xe_l = [dscr("s_xe%d" % i, [E * CAP, D], BF16) for i in range(nchunks)]
    ye_l = [dscr("s_ye%d" % i, [E * CAP, D], F32) for i in range(nchunks)]
    r_xe_l, r_ye_l = [R("xe") for _ in range(nchunks)], [R("ye") for _ in range(nchunks)]
    bupT = PL.sb([128, E, 2 * KF], F32, 'bupT')
    ctx.dma('sp', 'const', bupT[:], b_up.rearrange("e (t p) -> p e t", p=128), writes=[bupT], allow_slow_non_contiguous=True)
    gat_l = [PL.sb([128, NT, 4], F32, 'gat') for _ in range(nchunks)]
    sloti_l = [PL.sb([128, NT * 4], I32, 'sloti') for _ in range(nchunks)]
    for ch in range(nchunks):
        b = (ch * CH) // S
        roff = (ch * CH) % S
        gat, sloti, xe_c, r_xe_c = gat_l[ch], sloti_l[ch], xe_l[ch], r_xe_l[ch]
        P = Pool_(ctx)
        ecap = P.sb([128, E], F32, 'ecap')
        ctx.dma('sp', 'const', ecap[:], c_ecap[:, :], writes=[ecap])
        top8 = P.sb([128, NT, 8], F32, 'top8')
        Mall = P.sb([128, NT, E], BF16, 'Mall')
        Mf = P.sb([128, NT, E], F32, 'Mf')
        OH = P.sb([128, 4, NT, E], F32, 'OH')
        posf = P.sb([128, NT, E], F32, 'posf')
        basef = P.sb([128, NT, E], F32, 'basef')
        slotf = P.sb([128, NT, 4], F32, 'slotf')
        tmpE = P.sb([128, NT, E], F32, 'tmpE')
        negm = P.sb([128, NT], F32, 'negm')
        den = P.sb([128, NT], F32, 'den')
        psR = P.ring(2, [128, NT * E], F32, 'psR', psum=True)
        for tt in range(NT):
            gi = ch * NT + tt
            lg = lg_all[:, gi, :]
            ctx.op('dve', lambda g: g.max(out=top8[:, tt, :], in_=lg), reads=[lg_all], writes=[top8], disjoint=(tt > 0))
            ctx.op('dve', lambda g: g.tensor_single_scalar(out=Mf[:, tt, :], in_=lg, scalar=top8[:, tt, 3:4], op=ALU.is_ge),
                   reads=[lg_all, top8], writes=[Mf], disjoint=(tt > 0))
            for k in range(4):
                ctx.op('dve', lambda g: g.tensor_single_scalar(out=OH[:, k, tt, :], in_=lg, scalar=top8[:, tt, k:k + 1], op=ALU.is_equal),
                       reads=[lg_all, top8], writes=[OH], disjoint=(tt > 0 or k > 0))
            ctx.op('dve', lambda g: g.tensor_single_scalar(out=negm[:, tt:tt + 1], in_=top8[:, tt, 0:1], scalar=-1.0, op=ALU.mult),
                   reads=[top8], writes=[negm], disjoint=(tt > 0))
            ctx.op('act', lambda g: g.activation(out=gat[:, tt, :], in_=top8[:, tt, 0:4], func=AF.Exp, bias=negm[:, tt:tt + 1], scale=1.0,
                                                 accum_out=den[:, tt:tt + 1]), reads=[top8, negm], writes=[gat, den], disjoint=(tt > 0))
            ctx.op('dve', lambda g: g.reciprocal(out=den[:, tt:tt + 1], in_=den[:, tt:tt + 1]), reads=[den], writes=[den], disjoint=True)
            ctx.op('dve', lambda g: g.tensor_single_scalar(out=gat[:, tt, :], in_=gat[:, tt, :], scalar=den[:, tt:tt + 1], op=ALU.mult),
                   reads=[gat, den], writes=[gat], disjoint=True)
        ctx.op('dve', lambda g: g.tensor_copy(out=Mall[:], in_=Mf[:]), reads=[Mf], writes=[Mall])
        Mflat = Mall[:, :, :].rearrange("p t e -> p (t e)")
        ps1, ps2 = psR.next(), psR.next()
        ctx.op('pe', lambda g: g.matmul(ps1[:], lhsT=trib[:, 128:256], rhs=Mflat, start=True, stop=True), reads=[trib, Mall], writes=[ps1])
        ctx.op('pe', lambda g: g.matmul(ps2[:], lhsT=trib[:, 256:384], rhs=Mflat, start=True, stop=True), reads=[trib, Mall], writes=[ps2])
        ps2v = ps2[:, :].rearrange("p (t e) -> p t e", e=E)
        ctx.op('dve', lambda g: g.memset(basef[:, 0, :], 0.0), writes=[basef])
        for tt in range(1, NT):
            ctx.op('dve', lambda g: g.tensor_tensor(out=basef[:, tt, :], in0=basef[:, tt - 1, :], in1=ps2v[:, tt - 1, :], op=ALU.add),
                   reads=[basef, ps2], writes=[basef], disjoint=True)
        ctx.op('dve', lambda g: g.tensor_tensor(out=posf[:], in0=basef[:], in1=ps1[:, :].rearrange("p (t e) -> p t e", e=E), op=ALU.add),
               reads=[basef, ps1], writes=[posf])
        ctx.op('dve', lambda g: g.tensor_single_scalar(out=posf[:], in_=posf[:], scalar=float(CAP - 1), op=ALU.min), reads=[posf], writes=[posf])
        for tt in range(NT):
            ctx.op('dve', lambda g: g.tensor_tensor(out=posf[:, tt, :], in0=posf[:, tt, :], in1=ecap[:], op=ALU.add), reads=[posf, ecap], writes=[posf], disjoint=True)
        for k in range(4):
            ctx.op('dve', lambda g: g.tensor_tensor(out=tmpE[:], in0=OH[:, k, :, :], in1=posf[:], op=ALU.mult), reads=[OH, posf], writes=[tmpE])
            ctx.op('dve', lambda g: g.reduce_sum(out=slotf[:, :, k], in_=tmpE[:], axis=AX.X), reads=[tmpE], writes=[slotf], disjoint=(k > 0))
        ctx.op('dve', lambda g: g.tensor_copy(out=sloti[:], in_=slotf[:, :, :].rearrange("p t k -> p (t k)")), reads=[slotf], writes=[sloti])
        hbt_r = P.ring(3, [128, D], BF16, 'hbt')
        for tt in range(NT):
            hbt = hbt_r.next()
            rows = slice(roff + tt * 128, roff + (tt + 1) * 128)
            ctx.dma('sp', 'd_in', hbt[:], hb[b][rows, :], reads=[r_hb], writes=[hbt], disjoint=False)
            for k in range(4):
                col = tt * 4 + k
                ctx.dma('pool', 'scat', None, None, reads=[hbt, sloti], writes=[r_xe_c],
                        fn=lambda g: g.indirect_dma_start(out=xe_c[:, :], out_offset=bass.IndirectOffsetOnAxis(ap=sloti[:, col:col + 1], axis=0),
                                                          in_=hbt[:], in_offset=None))
        P.close()
    P = Pool_(ctx)
    xs_r = P.ring(1, [128, D], BF16, 'xs')
    xeT_l = [P.sb([128, KC, CAP], BF16, 'xeT') for _ in range(nchunks)]
    aT_l = [P.sb([128, KF, CAP], BF16, 'aT') for _ in range(nchunks)]
    stg_r = P.ring(3, [128, 4096], F32, 'stg')
    wb_r = P.ring(2, [128, 4096], BF16, 'wb')
    w2_r = P.ring(2, [128, KC * 256], BF16, 'w2')
    bd_r = P.ring(2, [128, 256], F32, 'bd')
    y_r = P.ring(3, [128, 256], F32, 'y')
    e1_r, e2_r, e3_r = P.ring(2, [128, CAP], F32, 'e1'), P.ring(2, [128, CAP], F32, 'e2'), P.ring(2, [128, CAP], F32, 'e3')
    psX = P.ring(2, [128, 1024], BF16, 'psX', psum=True)
    psU = P.ring(4, [128, 512], F32, 'psU', psum=True)
    psD = P.ring(2, [128, 512], F32, 'psD', psum=True)
    assert KC * 128 == 4096 or KC * 128 <= 4096
    ccnt = [0]

    def load_cast(src_ap, nk, ncol):
        st_, wb_ = stg_r.next(), wb_r.next()
        sv = st_[:, 0:nk * ncol].rearrange("p (k n) -> p k n", n=ncol)
        wv = wb_[:, 0:nk * ncol].rearrange("p (k n) -> p k n", n=ncol)
        ctx.dma('sp', 'e_w', sv, src_ap, writes=[st_], disjoint=False)
        eng = ('act', 'dve', 'act')[ccnt[0] % 3]
        ccnt[0] += 1
        if eng == 'act':
            ctx.op('act', lambda g: g.copy(out=wb_[:, 0:nk * ncol], in_=st_[:, 0:nk * ncol]), reads=[st_], writes=[wb_])
        else:
            ctx.op('dve', lambda g: g.tensor_copy(out=wb_[:, 0:nk * ncol], in_=st_[:, 0:nk * ncol]), reads=[st_], writes=[wb_])
        return wb_, wv

    for e_ in range(E):
        wu_e = w_up[e_ * D:(e_ + 1) * D, :].rearrange("(kc p) n -> p kc n", p=128)
        wd_e = w_down[e_ * DFF:(e_ + 1) * DFF, :].rearrange("(kc p) n -> p kc n", p=128)
        for ch in range(nchunks):
            xT_e = xeT_l[ch]
            for st_i in range(NS):
                xs = xs_r.next()
                r0 = e_ * CAP + st_i * 128
                ctx.dma('sp', 'e_in', xs[:], xe_l[ch][r0:r0 + 128, :], reads=[r_xe_l[ch]], writes=[xs], disjoint=False)
                for k4 in range(KC // 4):
                    ps = psX.next()
                    for kk in range(4):
                        kc = k4 * 4 + kk
                        ctx.op('pe', lambda g: g.transpose(out=ps[:, kk * 128:(kk + 1) * 128], in_=xs[:, kc * 128:(kc + 1) * 128], identity=identb[:]),
                               reads=[xs, identb], writes=[ps], disjoint=(kk > 0))
                    dst = xT_e[:, k4 * 4:(k4 + 1) * 4, st_i * 128:(st_i + 1) * 128]
                    src = ps[:, 0:512].rearrange("p (k t) -> p k t", k=4)
                    ctx.op('dve', lambda g: g.tensor_copy(out=dst, in_=src), reads=[ps], writes=[xT_e], disjoint=not (st_i == 0 and k4 == 0))
        KH = KC // 2

        def load_pair(col0):
            w2 = w2_r.next()
            for hh in range(2):
                st_ = stg_r.next()
                sv = st_[:, 0:KH * 256].rearrange("p (k n) -> p k n", n=256)
                ctx.dma('sp', 'e_w', sv, wu_e[:, hh * KH:(hh + 1) * KH, col0:col0 + 256], writes=[st_], disjoint=False)
                eng = ('act', 'dve', 'act')[ccnt[0] % 3]
                ccnt[0] += 1
                dst = w2[:, hh * KH * 256:(hh + 1) * KH * 256]
                if eng == 'act':
                    ctx.op('act', lambda g: g.copy(out=dst, in_=st_[:, 0:KH * 256]), reads=[st_], writes=[w2], disjoint=(hh > 0))
                else:
                    ctx.op('dve', lambda g: g.tensor_copy(out=dst, in_=st_[:, 0:KH * 256]), reads=[st_], writes=[w2], disjoint=(hh > 0))
            return w2, w2[:, :].rearrange("p (k n) -> p k n", n=256)

        for j in range(KF):
            if j % 2 == 0:
                wgR, wg2 = load_pair(j * 128)
                wuR, wu2 = load_pair(DFF + j * 128)
            cs_ = slice((j % 2) * 128, (j % 2 + 1) * 128)
            wg, wu = wg2[:, :, cs_], wu2[:, :, cs_]
            for ch in range(nchunks):
                xT_e, aT = xeT_l[ch], aT_l[ch]
                pg, pu = psU.next(), psU.next()
                for (ps, wR, wt) in ((pg, wgR, wg), (pu, wuR, wu)):
                    for kc in range(KC):
                        ctx.op('pe', lambda g: g.matmul(ps[:, :CAP], lhsT=wt[:, kc, :], rhs=xT_e[:, kc, :], start=(kc == 0), stop=(kc == KC - 1)),
                               reads=[wR, xT_e], writes=[ps], disjoint=(kc > 0))
                e1, e2, e3 = e1_r.next(), e2_r.next(), e3_r.next()
                ctx.op('dve', lambda g: g.tensor_scalar(out=e1[:], in0=pg[:, :CAP], scalar1=bupT[:, e_, j:j + 1], scalar2=7.0, op0=ALU.add, op1=ALU.min),
                       reads=[pg, bupT], writes=[e1])
                ctx.op('act', lambda g: g.activation(out=e2[:], in_=e1[:], func=AF.Sigmoid, scale=1.702), reads=[e1], writes=[e2])
                ctx.op('dve', lambda g: g.tensor_scalar(out=e3[:], in0=pu[:, :CAP], scalar1=bupT[:, e_, KF + j:KF + j + 1], scalar2=7.0, op0=ALU.add, op1=ALU.min),
                       reads=[pu, bupT], writes=[e3])
                ctx.op('pool', lambda g: g.tensor_scalar(out=e3[:], in0=e3[:], scalar1=-7.0, scalar2=1.0, op0=ALU.max, op1=ALU.add), reads=[e3], writes=[e3])
                ctx.op('pool', lambda g: g.tensor_tensor(out=e1[:], in0=e1[:], in1=e2[:], op=ALU.mult), reads=[e1, e2], writes=[e1])
                ctx.op('dve', lambda g: g.tensor_tensor(out=aT[:, j, :], in0=e1[:], in1=e3[:], op=ALU.mult), reads=[e1, e3], writes=[aT], disjoint=(j > 0))
        for cb in range(D // 256):
            wdR, wd = load_cast(wd_e[:, :, cb * 256:(cb + 1) * 256], KF, 256)
            bd = bd_r.next()
            ctx.dma('sp', 'e_w', bd[:], b_down[e_:e_ + 1, cb * 256:(cb + 1) * 256].partition_broadcast(128), writes=[bd], disjoint=False)
            for ch in range(nchunks):
                aT = aT_l[ch]
                for st_i in range(NS):
                    ps = psD.next()
                    for kc in range(KF):
                        ctx.op('pe', lambda g: g.matmul(ps[:, 0:256], lhsT=aT[:, kc, st_i * 128:(st_i + 1) * 128], rhs=wd[:, kc, :], start=(kc == 0), stop=(kc == KF - 1)),
                               reads=[aT, wdR], writes=[ps], disjoint=(kc > 0))
                    y = y_r.next()
                    ctx.op('dve', lambda g: g.tensor_tensor(out=y[:], in0=ps[:, 0:256], in1=bd[:], op=ALU.add), reads=[ps, bd], writes=[y])
                    r0 = e_ * CAP + st_i * 128
                    ctx.dma('act', 'y_out', ye_l[ch][r0:r0 + 128, cb * 256:(cb + 1) * 256], y[:], reads=[y], writes=[r_ye_l[ch]])
    P.close()
    P = Pool_(ctx)
    g2B, b2B = P.sb([128, D], F32, 'g2B'), P.sb([128, D], F32, 'b2B')
    ctx.dma('sp', 'const', g2B[:], ln2_g[0:1, :].partition_broadcast(128), writes=[g2B])
    ctx.dma('sp', 'const', b2B[:], ln2_b[0:1, :].partition_broadcast(128), writes=[b2B])
    yg_r = P.ring(3, [128, D], F32, 'yg')
    acc_r = P.ring(2, [128, D], F32, 'acc')
    hr_r = P.ring(2, [128, D], F32, 'hr')
    st_r, mv_r = P.ring(2, [128, 48], F32, 'st'), P.ring(2, [128, 4], F32, 'mv')
    for ch in range(nchunks):
        b = (ch * CH) // S
        roff = (ch * CH) % S
        gat, sloti, ye_c, r_ye_c = gat_l[ch], sloti_l[ch], ye_l[ch], r_ye_l[ch]
        for tt in range(NT):
            acc = acc_r.next()
            for k in range(4):
                yg = yg_r.next()
                col = tt * 4 + k
                ctx.dma('pool', 'gath', None, None, reads=[r_ye_c, sloti], writes=[yg], disjoint=False,
                        fn=lambda g: g.indirect_dma_start(out=yg[:], out_offset=None, in_=ye_c[:, :],
                                                          in_offset=bass.IndirectOffsetOnAxis(ap=sloti[:, col:col + 1], axis=0)))
                if k == 0:
                    ctx.op('dve', lambda g: g.tensor_single_scalar(out=acc[:], in_=yg[:], scalar=gat[:, tt, 0:1], op=ALU.mult),
                           reads=[yg, gat], writes=[acc])
                else:
                    ctx.op('dve', lambda g: g.scalar_tensor_tensor(out=acc[:], in0=yg[:], scalar=gat[:, tt, k:k + 1], in1=acc[:], op0=ALU.mult, op1=ALU.add),
                           reads=[yg, gat, acc], writes=[acc])
            hr = hr_r.next()
            rows = slice(roff + tt * 128, roff + (tt + 1) * 128)
            ctx.dma('sp', 'c_in', hr[:], h1[b][rows, :], reads=[r_h1], writes=[hr], disjoint=False)
            ctx.op('dve', lambda g: g.scalar_tensor_tensor(out=acc[:], in0=hr[:], scalar=c.alpha, in1=acc[:], op0=ALU.mult, op1=ALU.add),
                   reads=[hr, acc], writes=[acc])
            layer_norm(P, acc, g2B, b2B, st_r, mv_r)
            orow = ch * CH + tt * 128
            ctx.dma('act', 'out', out[orow:orow + 128, :], acc[:], reads=[acc], writes=[r_out])
    P.close()
    PL.close()
    P0.close()
    ctx.barrier()
    es.close()
    return nc, ctx


def host_consts(cfg):
    E, CAP = cfg.E, cfg.CAP
    cs = {}
    cs["c_identb"] = np.eye(128, dtype=np.float32).astype(ml_dtypes.bfloat16)
    cs["c_identf"] = np.eye(128, dtype=np.float32)
    q = np.arange(128)[:, None]
    k = np.arange(512)[None, :]
    mf = np.zeros((128, 4 * 512), np.float32)
    ms = np.zeros((128, 4 * 512), np.float32)
    for im in range(4):
        qpos = im * 128 + q
        mf[:, im * 512:(im + 1) * 512] = np.where(k <= qpos, 0.0, -30000.0)
        ms[:, im * 512:(im + 1) * 512] = np.where(k < qpos, 1.0, 0.0)
    cs["c_mfx"], cs["c_msb"] = mf, ms
    j = np.arange(128)[:, None]
    i = np.arange(128)[None, :]
    cs["c_tri"] = np.concatenate([(j <= i), (j < i), np.ones((128, 128), bool)], axis=1).astype(np.float32)
    cs["c_ecap"] = np.tile((np.arange(E) * CAP).astype(np.float32)[None, :], (128, 1))
    return cs


def run(cfg, ncores, inputs, trace=False, debug=False):
    nc, ctx = build(cfg, ncores, debug)
    c = cfg
    T, D, E, DFF = c.T, c.D, c.E, c.DFF
    cs = host_consts(cfg)
    f = lambda a: np.ascontiguousarray(np.asarray(a, dtype=np.float32))
    xs = f(inputs["x"]).reshape(ncores, T, D)
    shared = {
        "w_in": f(inputs["w_in"])[0], "b_in": f(inputs["b_in"]).reshape(1, -1),
        "w_branch_sb": f(inputs["w_branch_sb"])[0], "w_branch_fx": f(inputs["w_branch_fx"])[0],
        "w_out": f(inputs["w_out"])[0], "ln1_g": f(inputs["ln1_g"]).reshape(1, -1), "ln1_b": f(inputs["ln1_b"]).reshape(1, -1),
        "w_router": f(inputs["w_router"])[0], "b_router": f(inputs["b_router"]).reshape(1, -1),
        "w_up": f(inputs["w_up"]).reshape(E * D, 2 * DFF), "b_up": f(inputs["b_up"]).reshape(E, 2 * DFF),
        "w_down": f(inputs["w_down"]).reshape(E * DFF, D), "b_down": f(inputs["b_down"]).reshape(E, D),
        "ln2_g": f(inputs["ln2_g"]).reshape(1, -1), "ln2_b": f(inputs["ln2_b"]).reshape(1, -1),
    }
    shared.update(cs)
    in_maps = [dict(shared, x=xs[i]) for i in range(ncores)]
    res = run_bass_kernel_spmd(nc, in_maps, core_ids=list(range(ncores)), **({"trace": True} if trace else {}))
    outs = [np.asarray(r["out"], dtype=np.float32) for r in res.results]
    return np.concatenate(outs, axis=0), res


def kernel(**inputs):
    B, S, D = inputs["x"].shape
    cfg = Cfg(D=D, S=S, BL=B // NCORES)
    o, _ = run(cfg, NCORES, inputs)
    return o.reshape(B, S, D)
```
